# Optimizing a Trainium2 kernel written in Bass

```python
import math
import jax, jax.numpy as jnp
from jax import lax
import numpy as np

D_MODEL = 1024
BATCH = 8
SEQ = 2048
DEPTH = 2

CTX_LEN = 256
GRID_W = 64
HEAD_DIM = 64
AXIS_DIM = HEAD_DIM // 2
ROPE_BASE = 10000.0
RMS_EPS = 1e-6
Q_BLOCK = 128
WINDOW = 128

A_HEADS = 4
A_QK_DIM = 2 * HEAD_DIM
A_V_DIM = 2 * HEAD_DIM
B_HEADS = 4
B_KV_HEADS = 2
B_GROUP = B_HEADS // B_KV_HEADS
C_HEADS = 4
C_KV_HEADS = 2
C_GROUP = C_HEADS // C_KV_HEADS

A_WIDTH = A_HEADS * A_V_DIM
B_WIDTH = B_HEADS * HEAD_DIM
C_WIDTH = C_HEADS * HEAD_DIM
MIX_WIDTH = A_WIDTH + B_WIDTH + C_WIDTH
Q_SIZES = (A_HEADS * A_QK_DIM, B_HEADS * HEAD_DIM, C_HEADS * HEAD_DIM)
KV_SIZES = (A_HEADS * A_QK_DIM, A_HEADS * A_V_DIM,
            B_KV_HEADS * HEAD_DIM, B_KV_HEADS * HEAD_DIM,
            C_KV_HEADS * HEAD_DIM, C_KV_HEADS * HEAD_DIM)
Q_COLS = sum(Q_SIZES)
KV_COLS = sum(KV_SIZES)
IN_COLS = Q_COLS + KV_COLS

N_EXPERTS = 16
N_GROUPS = 4
EXPERTS_PER_GROUP = N_EXPERTS // N_GROUPS
TOP_K = 2
D_EXPERT = 256

kernel_name = 'hybrid_dit_parallel_heads_moe'


def _offsets(sizes):
    out, acc = [], 0
    for s in sizes[:-1]:
        acc += s
        out.append(acc)
    return out


def rms_norm(x, g):
    xf = x.astype(jnp.float32)
    y = xf * lax.rsqrt(jnp.mean(xf * xf, axis=-1, keepdims=True) + RMS_EPS)
    return (y * g.astype(jnp.float32)).astype(x.dtype)


def rope_tables(seq_len):
    rows = seq_len // GRID_W
    row_pos = jnp.repeat(jnp.arange(rows, dtype=jnp.float32), GRID_W)
    col_pos = jnp.tile(jnp.arange(GRID_W, dtype=jnp.float32), rows)
    inv_freq = ROPE_BASE ** (-jnp.arange(0, AXIS_DIM, 2, dtype=jnp.float32) / AXIS_DIM)
    ang_r = row_pos[:, None] * inv_freq[None, :]
    ang_c = col_pos[:, None] * inv_freq[None, :]
    return (jnp.cos(ang_r), jnp.sin(ang_r), jnp.cos(ang_c), jnp.sin(ang_c))


def _rot(x, cos, sin):
    x1, x2 = jnp.split(x, 2, axis=-1)
    cos = cos.astype(x.dtype)
    sin = sin.astype(x.dtype)
    return jnp.concatenate([x1 * cos - x2 * sin, x1 * sin + x2 * cos], axis=-1)


def apply_rope_2d(x, rope):
    cos_r, sin_r, cos_c, sin_c = rope
    x_row, x_col = jnp.split(x, 2, axis=-1)
    return jnp.concatenate([_rot(x_row, cos_r, sin_r), _rot(x_col, cos_c, sin_c)], axis=-1)


def to_query_blocks(q):
    *lead, s, d = q.shape
    return jnp.moveaxis(q.reshape(*lead, s // Q_BLOCK, Q_BLOCK, d), -3, 0)


def from_query_blocks(o):
    o = jnp.moveaxis(o, 0, -3)
    *lead, nb, qb, d = o.shape
    return o.reshape(*lead, nb * qb, d)


def diff_attention(q, k, v, lam):
    kf = k.astype(jnp.float32)
    scale = HEAD_DIM ** -0.5

    def block(qb):
        s = jnp.einsum('bhiqd,bhikd->bhiqk', qb.astype(jnp.float32), kf) * scale
        p = jax.nn.softmax(s, axis=-1)
        w = p[:, :, 0] - lam * p[:, :, 1]
        return jnp.einsum('bhqk,bhkv->bhqv', w.astype(v.dtype), v)

    return from_query_blocks(lax.map(block, to_query_blocks(q)))


def gqa_attention(q, k, v, sink=None):
    kf = k.astype(jnp.float32)
    scale = HEAD_DIM ** -0.5

    def block(qb):
        s = jnp.einsum('bhgqd,bhkd->bhgqk', qb.astype(jnp.float32), kf) * scale
        if sink is not None:
            sk = jnp.broadcast_to(sink.astype(jnp.float32)[None, :, :, None, None], s.shape[:-1] + (1,))
            p = jax.nn.softmax(jnp.concatenate([s, sk], axis=-1), axis=-1)[..., :-1]
        else:
            p = jax.nn.softmax(s, axis=-1)
        return jnp.einsum('bhgqk,bhkd->bhgqd', p.astype(v.dtype), v)

    return from_query_blocks(lax.map(block, to_query_blocks(q)))


def window_attention(q, k, v, kc, vc, sink):
    B, Hkv, G, S, dh = q.shape
    nb = S // Q_BLOCK
    L = kc.shape[2]

    def band(t):
        tp = jnp.pad(t, ((0, 0), (0, 0), (WINDOW, WINDOW), (0, 0)))
        tp = tp.reshape(B, Hkv, nb + 2, Q_BLOCK, t.shape[-1])
        return jnp.concatenate([tp[:, :, :-2], tp[:, :, 1:-1], tp[:, :, 2:]], axis=3)

    kb, vb = band(k), band(v)
    qb = q.reshape(B, Hkv, G, nb, Q_BLOCK, dh).astype(jnp.float32)
    scale = HEAD_DIM ** -0.5
    s_band = jnp.einsum('bhgnqd,bhnkd->bhgnqk', qb, kb.astype(jnp.float32)) * scale
    blk = jnp.arange(nb)[:, None, None] * Q_BLOCK
    q_pos = blk + jnp.arange(Q_BLOCK)[None, :, None]
    k_pos = blk - WINDOW + jnp.arange(3 * Q_BLOCK)[None, None, :]
    valid = (jnp.abs(k_pos - q_pos) <= WINDOW) & (k_pos >= 0) & (k_pos < S)
    s_band = jnp.where(valid, s_band, -jnp.inf)
    s_ctx = jnp.einsum('bhgnqd,bhkd->bhgnqk', qb, kc.astype(jnp.float32)) * scale
    s_sink = jnp.broadcast_to(sink.astype(jnp.float32)[None, :, :, None, None, None], s_ctx.shape[:-1] + (1,))
    p = jax.nn.softmax(jnp.concatenate([s_ctx, s_band, s_sink], axis=-1), axis=-1)
    p_ctx = p[..., :L].astype(v.dtype)
    p_band = p[..., L:L + 3 * Q_BLOCK].astype(v.dtype)
    out = (jnp.einsum('bhgnqk,bhkd->bhgnqd', p_ctx, vc)
           + jnp.einsum('bhgnqk,bhnkd->bhgnqd', p_band, vb))
    return out.reshape(B, Hkv, G, S, dh)


def heads_a_qk(t):
    B, n, _ = t.shape
    return t.reshape(B, n, A_HEADS, 2, HEAD_DIM).transpose(0, 2, 3, 1, 4)


def heads_kv(t, heads, dim):
    B, n, _ = t.shape
    return t.reshape(B, n, heads, dim).transpose(0, 2, 1, 3)


def heads_gq(t, hkv, g):
    B, n, _ = t.shape
    return t.reshape(B, n, hkv, g, HEAD_DIM).transpose(0, 2, 3, 1, 4)


def merge_h(o):
    B, H, n, d = o.shape
    return o.transpose(0, 2, 1, 3).reshape(B, n, H * d)


def merge_gq(o):
    B, Hkv, G, n, d = o.shape
    return o.transpose(0, 3, 1, 2, 4).reshape(B, n, Hkv * G * d)


def split_kv(kv):
    kA, vA, kB, vB, kC, vC = jnp.split(kv, _offsets(KV_SIZES), axis=-1)
    return (heads_a_qk(kA), heads_kv(vA, A_HEADS, A_V_DIM),
            heads_kv(kB, B_KV_HEADS, HEAD_DIM), heads_kv(vB, B_KV_HEADS, HEAD_DIM),
            heads_kv(kC, C_KV_HEADS, HEAD_DIM), heads_kv(vC, C_KV_HEADS, HEAD_DIM))


def split_q(q):
    qA, qB, qC = jnp.split(q, _offsets(Q_SIZES), axis=-1)
    return heads_a_qk(qA), heads_gq(qB, B_KV_HEADS, B_GROUP), heads_gq(qC, C_KV_HEADS, C_GROUP)


def token_mixers(hx, hc, w_in, lam_q1, lam_k1, lam_q2, lam_k2, subln_g, q_norm_g, k_norm_g,
                 sink, lambda_init, rope, with_ctx):
    px = hx @ w_in
    qAx, qBx, qCx = split_q(px[..., :Q_COLS])
    kAx, vAx, kBx, vBx, kCx, vCx = split_kv(px[..., Q_COLS:])
    kAc, vAc, kBc, vBc, kCc, vCc = split_kv(hc @ w_in[:, Q_COLS:])

    f32 = jnp.float32
    lam = (jnp.exp(jnp.sum(lam_q1.astype(f32) * lam_k1.astype(f32)))
           - jnp.exp(jnp.sum(lam_q2.astype(f32) * lam_k2.astype(f32))) + lambda_init)
    sink_hg = sink.reshape(C_KV_HEADS, C_GROUP)
    kBc = rms_norm(kBc, k_norm_g)

    oA = diff_attention(apply_rope_2d(qAx, rope),
                        jnp.concatenate([kAc, apply_rope_2d(kAx, rope)], axis=-2),
                        jnp.concatenate([vAc, vAx], axis=-2), lam)
    oA = rms_norm(oA, subln_g) * (1.0 - lambda_init)
    oB = gqa_attention(apply_rope_2d(rms_norm(qBx, q_norm_g), rope),
                       jnp.concatenate([kBc, apply_rope_2d(rms_norm(kBx, k_norm_g), rope)], axis=-2),
                       jnp.concatenate([vBc, vBx], axis=-2))
    oC = window_attention(apply_rope_2d(qCx, rope), apply_rope_2d(kCx, rope), vCx, kCc, vCc, sink_hg)
    mix_x = jnp.concatenate([merge_h(oA), merge_gq(oB), merge_gq(oC)], axis=-1)

    if not with_ctx:
        return mix_x, None
    qAc, qBc, qCc = split_q(hc @ w_in[:, :Q_COLS])
    oAc = rms_norm(diff_attention(qAc, kAc, vAc, lam), subln_g) * (1.0 - lambda_init)
    oBc = gqa_attention(rms_norm(qBc, q_norm_g), kBc, vBc)
    oCc = gqa_attention(qCc, kCc, vCc, sink_hg)
    mix_c = jnp.concatenate([merge_h(oAc), merge_gq(oBc), merge_gq(oCc)], axis=-1)
    return mix_x, mix_c


def moe_ffn(h, w_router, router_bias, w_gate, w_up, w_down):
    shape = h.shape
    t = h.reshape(-1, shape[-1])
    f32 = jnp.float32
    scores = jax.nn.sigmoid(t.astype(f32) @ w_router.astype(f32))
    sel = (scores + router_bias.astype(f32)).reshape(-1, N_GROUPS, EXPERTS_PER_GROUP)
    group_score = jnp.sum(lax.top_k(sel, 2)[0], axis=-1)
    best_group = jnp.argmax(group_score, axis=-1)
    in_group = jnp.take_along_axis(sel, best_group[:, None, None], axis=1)[:, 0]
    _, local = lax.top_k(in_group, TOP_K)
    expert_idx = best_group[:, None] * EXPERTS_PER_GROUP + local
    w = jnp.take_along_axis(scores, expert_idx, axis=-1)
    w = w / jnp.sum(w, axis=-1, keepdims=True)
    gates = jnp.sum(jax.nn.one_hot(expert_idx, N_EXPERTS, dtype=f32) * w[..., None], axis=1).astype(t.dtype)
    out = jnp.zeros_like(t)
    for e in range(N_EXPERTS):
        hid = jax.nn.silu(t @ w_gate[e]) * (t @ w_up[e])
        out = out + gates[:, e:e + 1] * (hid @ w_down[e])
    return out.reshape(shape)


def setup_inputs(seed: int = 0) -> dict:
    key = jax.random.key(seed)
    ks = jax.random.split(key, 24)
    f32 = jnp.float32
    D = D_MODEL

    def nrm(k, shape, s):
        return jax.random.normal(k, shape, f32) * s

    return {
        'x': nrm(ks[0], (BATCH, SEQ, D), 1.0),
        'c': nrm(ks[1], (BATCH, D), 1.0),
        'ctx': nrm(ks[2], (BATCH, CTX_LEN, D), 1.0),
        'c_ctx': nrm(ks[3], (D,), 1.0),
        'w_ada': nrm(ks[4], (DEPTH, D, 6 * D), 0.5 * D ** -0.5),
        'b_ada': nrm(ks[5], (DEPTH, 6 * D), 0.02),
        'norm1_g': 1.0 + nrm(ks[6], (DEPTH, D), 0.02),
        'norm2_g': 1.0 + nrm(ks[7], (DEPTH, D), 0.02),
        'w_in': nrm(ks[8], (DEPTH, D, IN_COLS), D ** -0.5),
        'w_out': nrm(ks[9], (DEPTH, MIX_WIDTH, D), MIX_WIDTH ** -0.5),
        'lam_q1': nrm(ks[10], (DEPTH, HEAD_DIM), 0.1),
        'lam_k1': nrm(ks[11], (DEPTH, HEAD_DIM), 0.1),
        'lam_q2': nrm(ks[12], (DEPTH, HEAD_DIM), 0.1),
        'lam_k2': nrm(ks[13], (DEPTH, HEAD_DIM), 0.1),
        'subln_g': 1.0 + nrm(ks[14], (DEPTH, A_V_DIM), 0.02),
        'q_norm_g': 1.0 + nrm(ks[15], (DEPTH, HEAD_DIM), 0.02),
        'k_norm_g': 1.0 + nrm(ks[16], (DEPTH, HEAD_DIM), 0.02),
        'sink': nrm(ks[17], (DEPTH, C_HEADS), 0.5),
        'w_router': nrm(ks[18], (D, N_EXPERTS), D ** -0.5),
        'router_bias': nrm(ks[19], (N_EXPERTS,), 0.01),
        'w_gate': nrm(ks[20], (DEPTH, N_EXPERTS, D, D_EXPERT), D ** -0.5),
        'w_up': nrm(ks[21], (DEPTH, N_EXPERTS, D, D_EXPERT), D ** -0.5),
        'w_down': nrm(ks[22], (DEPTH, N_EXPERTS, D_EXPERT, D), D_EXPERT ** -0.5),
        'final_g': 1.0 + nrm(ks[23], (D,), 0.02),
    }


def reference(x, c, ctx, c_ctx, w_ada, b_ada, norm1_g, norm2_g, w_in, w_out,
              lam_q1, lam_k1, lam_q2, lam_k2, subln_g, q_norm_g, k_norm_g, sink,
              w_router, router_bias, w_gate, w_up, w_down, final_g):
    rope = rope_tables(x.shape[1])
    silu_c = jax.nn.silu(c)
    silu_cc = jax.nn.silu(c_ctx)
    for l in range(DEPTH):
        last = l == DEPTH - 1
        lambda_init = 0.8 - 0.6 * math.exp(-0.3 * l)
        mod = jnp.split(silu_c @ w_ada[l] + b_ada[l], 6, axis=-1)
        sh1, sc1, g1, sh2, sc2, g2 = [m[:, None, :] for m in mod]
        csh1, csc1, cg1, csh2, csc2, cg2 = jnp.split(silu_cc @ w_ada[l] + b_ada[l], 6, axis=-1)

        hx = rms_norm(x, norm1_g[l]) * (1 + sc1) + sh1
        hc = rms_norm(ctx, norm1_g[l]) * (1 + csc1) + csh1
        mix_x, mix_c = token_mixers(hx, hc, w_in[l], lam_q1[l], lam_k1[l], lam_q2[l], lam_k2[l],
                                    subln_g[l], q_norm_g[l], k_norm_g[l], sink[l],
                                    lambda_init, rope, not last)
        x = x + g1 * (mix_x @ w_out[l])
        hx = rms_norm(x, norm2_g[l]) * (1 + sc2) + sh2
        x = x + g2 * moe_ffn(hx, w_router, router_bias, w_gate[l], w_up[l], w_down[l])

        if not last:
            ctx = ctx + cg1 * (mix_c @ w_out[l])
            hc = rms_norm(ctx, norm2_g[l]) * (1 + csc2) + csh2
            ctx = ctx + cg2 * moe_ffn(hc, w_router, router_bias, w_gate[l], w_up[l], w_down[l])
    return rms_norm(x, final_g)
```

```python
import contextlib
import math
import numpy as np
import concourse.bass as bass
import concourse.mybir as mybir
from concourse.bass_utils import run_bass_kernel_spmd
from concourse.alu_op_type import AluOpType as ALU

AF = mybir.ActivationFunctionType
F32 = mybir.dt.float32
BF16 = mybir.dt.bfloat16
AX = mybir.AxisListType

D = 1024
SEQ = 2048
CTXL = 256
T = SEQ + CTXL
NT = 18
NTL = 16
DEPTH = 2
NE = 16
DEXP = 256
RMS_EPS = 1e-6
SBUF_BYTES = 212000


class Dep:
    __slots__ = ("name", "w", "r", "sem", "cnt")

    def __init__(self, name):
        self.name = name
        self.w = None
        self.r = {}
        self.sem = None
        self.cnt = 0


class Sched:
    ENGS = ("pe", "act", "dve", "pool", "sp")

    def __init__(self, nc, stack):
        self.nc = nc
        self.stack = stack
        self.streams = {e: [] for e in self.ENGS}
        self.count = {e: 0 for e in self.ENGS}
        self.seen = {e: {} for e in self.ENGS}
        self.semh = {}
        self.nsem = 0
        self.dma_deps = []
        for e in self.ENGS:
            self.semh[e] = stack.enter_context(nc.semaphore("s_" + e))
            self.nsem += 1
        self.ninstr = 0

    def new_sem(self, name):
        key = "d%d_%s" % (self.nsem, name)
        self.semh[key] = self.stack.enter_context(self.nc.semaphore(key))
        self.nsem += 1
        return key

    def _waits(self, eng, reads, writes, banks=()):
        need = {}
        for d in banks:
            if d.w is not None and d.w[0] != eng:
                s, v = d.w
                if need.get(s, 0) < v:
                    need[s] = v
        for d in reads:
            if d.w is not None:
                s, v = d.w
                if need.get(s, 0) < v:
                    need[s] = v
        for d in writes:
            if d.w is not None:
                s, v = d.w
                if need.get(s, 0) < v:
                    need[s] = v
            for s, v in d.r.items():
                if need.get(s, 0) < v:
                    need[s] = v
        out = []
        seen = self.seen[eng]
        for s, v in need.items():
            if s == eng and eng == "pe":
                continue
            if seen.get(s, 0) >= v:
                continue
            seen[s] = v
            out.append((s, v))
        return out

    def op(self, eng, fn, reads=(), writes=(), banks=()):
        waits = self._waits(eng, reads, writes, banks)
        self.count[eng] += 1
        val = self.count[eng]
        semh = self.semh
        esem = semh[eng]

        def emit(h, waits=waits, fn=fn):
            for s, v in waits:
                h.wait_ge(semh[s], v)
            fn(h).then_inc(esem, 1)

        self.streams[eng].append(emit)
        for d in reads:
            if d.r.get(eng, 0) < val:
                d.r[eng] = val
        for d in writes:
            d.w = (eng, val)
            d.r = {}
        for d in banks:
            d.w = (eng, val)
        self.ninstr += 1

    def dma(self, q, fn, reads=(), writes=(), own=None):
        if own is None:
            own = writes[0]
        if own.sem is None:
            own.sem = self.new_sem(own.name)
            self.dma_deps.append(own)
        waits = self._waits(q, reads, writes)
        own.cnt += 16
        val = own.cnt
        semh = self.semh
        osem = semh[own.sem]

        def emit(h, waits=waits, fn=fn):
            for s, v in waits:
                h.wait_ge(semh[s], v)
            fn(h).then_inc(osem, 16)

        self.streams[q].append(emit)
        for d in reads:
            if d.r.get(own.sem, 0) < val:
                d.r[own.sem] = val
        for d in writes:
            d.w = (own.sem, val)
            d.r = {}
        self.ninstr += 1

    def barrier(self):
        semh = self.semh
        waits = []
        seen = self.seen["sp"]
        for e in self.ENGS:
            if e != "sp" and seen.get(e, 0) < self.count[e]:
                waits.append((e, self.count[e]))
        for d in self.dma_deps:
            if seen.get(d.sem, 0) < d.cnt:
                waits.append((d.sem, d.cnt))
        self.count["sp"] += 1
        val = self.count["sp"]
        spsem = semh["sp"]

        def emit(h, waits=waits):
            for s, v in waits:
                h.wait_ge(semh[s], v)
            h.nop().then_inc(spsem, 1)

        self.streams["sp"].append(emit)
        snap = {e: self.count[e] for e in self.ENGS}
        for d in self.dma_deps:
            snap[d.sem] = d.cnt
        for e in self.ENGS:
            if e != "sp":
                self.streams[e].append(lambda h, val=val: h.wait_ge(spsem, val))
            self.seen[e] = dict(snap)

    def final_wait(self, eng, deps):
        waits = self._waits(eng, deps, ())
        semh = self.semh

        def emit(h, waits=waits):
            for s, v in waits:
                h.wait_ge(semh[s], v)

        self.streams[eng].append(emit)

    def run(self):
        nc = self.nc
        streams = self.streams
        with nc.Block() as block:
            @block.tensor
            def _(h):
                for f in streams["pe"]:
                    f(h)

            @block.scalar
            def _(h):
                for f in streams["act"]:
                    f(h)

            @block.vector
            def _(h):
                for f in streams["dve"]:
                    f(h)

            @block.gpsimd
            def _(h):
                for f in streams["pool"]:
                    f(h)

            @block.sync
            def _(h):
                for f in streams["sp"]:
                    f(h)


class Region:
    def __init__(self, sb, base, size):
        self.sb = sb
        self.base = base
        self.size = size
        self.off = 0

    def reset(self):
        self.off = 0

    def alloc(self, shape, dt):
        esz = 4 if dt == F32 else 2
        n = 1
        for s in shape[1:]:
            n *= s
        nbytes = (n * esz + 31) // 32 * 32
        assert self.off + nbytes <= self.size, ("region overflow", self.off, nbytes, self.size)
        b = self.base + self.off
        self.off += nbytes
        ap = self.sb[:, b // 2:(b + n * esz) // 2]
        if dt != BF16:
            ap = ap.bitcast(dt)
        if len(shape) == 3:
            ap = ap.rearrange("p (a b) -> p a b", a=shape[1])
        elif len(shape) == 4:
            ap = ap.rearrange("p (a b c) -> p a b c", a=shape[1], b=shape[2])
        if shape[0] != 128:
            ap = ap[0:shape[0]]
        return ap


def build_program(debug=False, nlayers=DEPTH, stop_after=None):
    nc = bass.Bass("TRN2", target_bir_lowering=False)
    din = lambda name, shape: nc.dram_tensor(name, shape, F32, kind="ExternalInput").ap()
    x_d = din("x", [SEQ, D])
    ctx_d = din("ctx", [CTXL, D])
    cc_d = din("cc", [128, 8, 2])
    w_ada_d = din("w_ada", [DEPTH, D, 6 * D])
    b_ada_d = din("b_ada", [DEPTH, 6 * D])
    ng_d = din("ng", [128, DEPTH, 2, 8])
    final_g_d = din("final_g", [D])
    w_in_d = din("w_in_p", [DEPTH, D, 2560])
    w_out_d = din("w_out", [DEPTH, D, D])
    lamv_d = din("lamv", [DEPTH, 256])
    subln_d = din("subln_g", [DEPTH, 128])
    qkn_d = din("qkn", [128, DEPTH, 4])
    sink_d = din("sink", [DEPTH, 4])
    w_router_d = din("w_router", [D, NE])
    rbias_d = din("router_bias", [NE])
    w_gate_d = din("w_gate", [DEPTH, NE, D, DEXP])
    w_up_d = din("w_up", [DEPTH, NE, D, DEXP])
    w_down_d = din("w_down", [DEPTH, NE, DEXP, D])
    ident_d = din("ident", [128, 128])
    ropeC_d = din("ropeC", [128, SEQ])
    ropeS_d = din("ropeS", [128, SEQ])
    mprev_d = din("mprev", [128, 128])
    mnext_d = din("mnext", [128, 128])
    blk64_d = din("blk64", [128, 128])
    out_d = nc.dram_tensor("out", [SEQ, D], F32, kind="ExternalOutput").ap()
    xsp_d = nc.dram_tensor("xsp", [SEQ, D], F32, kind="Internal").ap()
    dbg_outs = {}

    with contextlib.ExitStack() as st:
        S = Sched(nc, st)
        SB = st.enter_context(nc.sbuf_tensor("SB", [128, SBUF_BYTES // 2], BF16))
        PS = st.enter_context(nc.psum_tensor("PS", [128, 8, 512], F32))
        P_SIZE = 24576
        R2_SIZE = 36864
        R1_SIZE = 92736
        L_SIZE = SBUF_BYTES - P_SIZE - R2_SIZE - R1_SIZE
        RP = Region(SB, 0, P_SIZE)
        RR2 = Region(SB, P_SIZE, R2_SIZE)
        RR1 = Region(SB, P_SIZE + R2_SIZE, R1_SIZE)
        RL = Region(SB, P_SIZE + R2_SIZE + R1_SIZE, L_SIZE)

        def V(fn, r=(), w=(), b=()):
            S.op("dve", fn, r, w, b)

        def A(fn, r=(), w=(), b=()):
            S.op("act", fn, r, w, b)

        def PE(fn, r=(), w=(), b=()):
            S.op("pe", fn, r, w, b)

        def G(fn, r=(), w=()):
            S.op("pool", fn, r, w)

        def dump(name, ap, deps, dt=F32):
            if not debug:
                return
            shape = list(ap.shape)
            t = nc.dram_tensor("dbg_" + name, shape, dt, kind="ExternalOutput").ap()
            dd = Dep("dbg_" + name)
            dbg_outs[name] = dd
            S.dma("sp", lambda h: h.dma_start(out=t, in_=ap), reads=deps, writes=[dd])

        identf = RP.alloc([128, 128], F32)
        identb = RP.alloc([128, 128], BF16)
        mprevb = RP.alloc([128, 128], BF16)
        mnextb = RP.alloc([128, 128], BF16)
        mprevn = RP.alloc([128, 128], BF16)
        mnextn = RP.alloc([128, 128], BF16)
        blk64 = RP.alloc([128, 128], F32)
        mhalf = RP.alloc([128, 512], F32)
        epsD = RP.alloc([128, 1], F32)
        ccf = RP.alloc([128, 8, 2], F32)
        scT = RP.alloc([128, 8, 2], BF16)
        ng = RP.alloc([128, DEPTH, 2, 8], F32)
        bfm = RP.alloc([128, 48], F32)
        modfm = RP.alloc([128, 48, 2], F32)
        scale1 = RP.alloc([128, 8, 2], F32)
        scale2 = RP.alloc([128, 8, 2], F32)
        gx1 = RP.alloc([128, D], F32)
        gc1 = RP.alloc([128, D], F32)
        gx2 = RP.alloc([128, D], F32)
        gc2 = RP.alloc([128, D], F32)
        wrt = RP.alloc([128, 8, NE], BF16)
        rbias = RP.alloc([128, NE], F32)
        lamt = RP.alloc([128, 4, 64], F32)
        lamt_flat = lamt.rearrange('p a b -> p (a b)')
        lamj = RP.alloc([128, 64], F32)
        lams = RP.alloc([128, 2], F32)
        neglam = RP.alloc([128, 1], F32)
        gA = RP.alloc([128, 128], F32)
        qkn = RP.alloc([128, DEPTH, 4], F32)
        sinkt = RP.alloc([128, 4], F32)
        esink = RP.alloc([128, 4], F32)

        d_const = Dep("const")
        d_cc = Dep("cc")
        d_scT = Dep("scT")
        d_ng = Dep("ng")
        d_mod = Dep("mod")
        d_gates = [Dep("g%d" % i) for i in range(4)]
        d_wrt = Dep("wrt")
        d_rb = Dep("rb")
        d_lam = Dep("lam")
        d_gA = Dep("gA")
        d_qkn = Dep("qkn")
        d_sink = Dep("sink")
        d_fing = Dep("fing")
        d_hT = Dep("hT")
        d_xres = [Dep("xres%d" % t) for t in range(NT)]
        d_ps = [Dep("ps%d" % i) for i in range(8)]
        d_xsp = Dep("xsp")
        d_out = Dep("out")

        d_idf = Dep("idf")
        d_mpf = Dep("mpf")
        tmpc = RL.alloc([128, 3, 128], F32)
        S.dma("sp", lambda h: h.dma_start(out=identf, in_=ident_d), writes=[d_idf])
        S.dma("sp", lambda h: h.dma_start(out=tmpc[:, 0, :], in_=mprev_d), writes=[d_mpf])
        S.dma("sp", lambda h: h.dma_start(out=tmpc[:, 1, :], in_=mnext_d), writes=[d_mpf])
        S.dma("sp", lambda h: h.dma_start(out=blk64, in_=blk64_d), writes=[d_const])
        S.dma("sp", lambda h: h.dma_start(out=ccf, in_=cc_d), writes=[d_cc])
        S.dma("sp", lambda h: h.dma_start(out=ng, in_=ng_d), writes=[d_ng])
        S.dma("sp", lambda h: h.dma_start(out=qkn, in_=qkn_d), writes=[d_qkn])
        S.dma("sp", lambda h: h.dma_start(out=rbias, in_=rbias_d.partition_broadcast(128)), writes=[d_rb])
        S.dma("pool", lambda h: h.dma_start(out=wrt, in_=w_router_d.rearrange("(kc p) n -> p kc n", p=128)), writes=[d_wrt])
        V(lambda h: h.tensor_copy(out=identb, in_=identf), [d_idf], [d_const])
        V(lambda h: h.tensor_copy(out=mprevb, in_=tmpc[:, 0, :]), [d_mpf], [d_const])
        V(lambda h: h.tensor_copy(out=mnextb, in_=tmpc[:, 1, :]), [d_mpf], [d_const])
        V(lambda h: h.tensor_scalar(out=mprevn, in0=tmpc[:, 0, :], scalar1=-1.0, scalar2=30000.0, op0=ALU.add, op1=ALU.mult), [d_mpf], [d_const])
        V(lambda h: h.tensor_scalar(out=mnextn, in0=tmpc[:, 1, :], scalar1=-1.0, scalar2=30000.0, op0=ALU.add, op1=ALU.mult), [d_mpf], [d_const])
        V(lambda h: h.memset(mhalf, -0.5), [], [d_const])
        V(lambda h: h.memset(epsD, RMS_EPS), [], [d_const])
        A(lambda h: h.activation(out=ccf, in_=ccf, func=AF.Silu), [d_cc], [d_cc])
        V(lambda h: h.tensor_copy(out=scT, in_=ccf), [d_cc], [d_scT])
        S.barrier()

        def rms_to_featmajor(src_tile_fn, tiles_groups, scale_t, bias_lo, dst, d_dst, src_dep_fn, loader=None):
            RL.reset()
            junk = RL.alloc([128, D], BF16)
            yn = [RL.alloc([128, D], BF16) for _ in range(8)]
            ssq = RL.alloc([128, NT], F32)
            rst = RL.alloc([128, NT], F32)
            xs = [RL.alloc([128, D], F32) for _ in range(6)]
            d_xs = [Dep("xs%d" % i) for i in range(6)]
            nload = [0]
            d_junk = Dep("junk")
            d_yn = [Dep("yn%d" % i) for i in range(8)]
            d_ssq = [Dep("ssq%d" % i) for i in range(NT)]
            d_rst = [Dep("rst%d" % i) for i in range(NT)]
            def stage_a(gi):
                tiles = tiles_groups[gi]
                par = gi % 2
                srcs = []
                for i, t in enumerate(tiles):
                    if loader is not None:
                        k3 = nload[0] % 6
                        nload[0] += 1
                        loader(t, xs[k3], d_xs[k3])
                        src = xs[k3]
                        sd = d_xs[k3]
                    else:
                        src = src_tile_fn(t)
                        sd = src_dep_fn(t)
                    A(lambda h, src=src, t=t: h.activation(out=junk, in_=src, func=AF.Square, accum_out=ssq[:, t:t + 1]),
                      [sd], [d_junk, d_ssq[t]])
                    srcs.append((src, sd))
                ta_, tb2_ = tiles[0], tiles[-1] + 1
                A(lambda h: h.activation(out=rst[:, ta_:tb2_], in_=ssq[:, ta_:tb2_], func=AF.Sqrt, scale=1.0 / D, bias=epsD[:, 0:1]),
                  [d_ssq[t] for t in tiles] + [d_const], [d_rst[t] for t in tiles])
                V(lambda h: h.reciprocal(out=rst[:, ta_:tb2_], in_=rst[:, ta_:tb2_]), [d_rst[t] for t in tiles], [d_rst[t] for t in tiles])
                for i, t in enumerate(tiles):
                    src, sd = srcs[i]
                    yi = par * 4 + i
                    V(lambda h, src=src, t=t, yi=yi: h.tensor_scalar(out=yn[yi], in0=src, scalar1=rst[:, t:t + 1], scalar2=None, op0=ALU.mult),
                      [sd, d_rst[t]], [d_yn[yi]])

            def stage_b(gi):
                tiles = tiles_groups[gi]
                par = gi % 2
                n = len(tiles)
                col = 0 if tiles[0] < NTL else 1
                t0 = tiles[0]
                for c in range(8):
                    psb = PS[:, c, :].bitcast(BF16)
                    for i, t in enumerate(tiles):
                        yi = par * 4 + i
                        PE(lambda h, psb=psb, yi=yi, c=c, i=i: h.transpose(
                            out=psb[:, i * 128:(i + 1) * 128], in_=yn[yi][:, c * 128:(c + 1) * 128], identity=identb),
                           [d_yn[yi], d_const], [], [d_ps[c]])
                    src = psb[:, 0:n * 128]
                    dsl = dst[:, c, t0 * 128:(t0 + n) * 128]
                    if c % 2 == 0:
                        A(lambda h, src=src, dsl=dsl, c=c: h.activation(out=dsl, in_=src, func=AF.Identity,
                                                                        scale=scale_t[:, c, col:col + 1],
                                                                        bias=modfm[:, bias_lo + c, col:col + 1]),
                          [d_mod], [d_dst], [d_ps[c]])
                    else:
                        V(lambda h, src=src, dsl=dsl, c=c: h.tensor_scalar(out=dsl, in0=src, scalar1=scale_t[:, c, col:col + 1],
                                                                           scalar2=modfm[:, bias_lo + c, col:col + 1],
                                                                           op0=ALU.mult, op1=ALU.add),
                          [d_mod], [d_dst], [d_ps[c]])

            ng_ = len(tiles_groups)
            stage_a(0)
            for gi in range(ng_):
                if gi + 1 < ng_:
                    stage_a(gi + 1)
                stage_b(gi)

        hT = RR2.alloc([128, 8, T], BF16)

        GS = {}

        def layer(l):
            xres = GS.get('xres')
            last = (l == DEPTH - 1)
            lambda_init = 0.8 - 0.6 * math.exp(-0.3 * l)
            tok_tiles = list(range(NT)) if not last else list(range(NTL))
            RL.reset()
            wa = [RL.alloc([128, 8, 512], BF16) for _ in range(3)]
            scB = RL.alloc([128, 2, 8, 128], BF16)
            d_wa = [Dep("wa%d" % i) for i in range(3)]
            d_scB = Dep("scB")
            d_bfm = Dep("bfm")
            d_psfm = Dep("psfm")
            d_psbc = [Dep("psbc%d" % i) for i in range(4)]
            for i in range(6):
                S.dma("sp", lambda h, i=i: h.dma_start(out=bfm[:, i * 8:(i + 1) * 8],
                                                       in_=b_ada_d[l, i * 1024:(i + 1) * 1024].rearrange("(c p) -> p c", p=128),
                                                       allow_slow_non_contiguous=True), writes=[d_bfm])
            for gi, (gt, lo) in enumerate(((gx1, 2048), (gc1, 2048), (gx2, 5120), (gc2, 5120))):
                S.dma("sp", lambda h, gt=gt, lo=lo: h.dma_start(out=gt, in_=b_ada_d[l, lo:lo + 1024].partition_broadcast(128)),
                      writes=[d_gates[gi]])
            for j in range(2):
                V(lambda h, j=j: h.tensor_copy(out=scB[:, j, :, :], in_=scT[:, :, j:j + 1].to_broadcast([128, 8, 128])), [d_scT], [d_scB])
            psfm = PS[:, 0, 0:96]
            for j in range(12):
                wj = wa[j % 3]
                dwj = d_wa[j % 3]
                S.dma("pool", lambda h, wj=wj, j=j: h.dma_start(out=wj, in_=w_ada_d[l, :, j * 512:(j + 1) * 512].rearrange("(kc p) n -> p kc n", p=128)),
                      writes=[dwj])
                for f in range(4):
                    fc = j * 4 + f
                    for kc in range(8):
                        PE(lambda h, wj=wj, f=f, kc=kc, fc=fc: h.matmul(psfm[:, fc * 2:fc * 2 + 2], lhsT=wj[:, kc, f * 128:(f + 1) * 128],
                                                                        rhs=scT[:, kc, :], start=(kc == 0), stop=(kc == 7)),
                           [dwj, d_scT], [], [d_ps[0]])
                if j in (4, 5, 10, 11):
                    half = j % 2 if j < 6 else (j - 10)
                    for who in range(2):
                        if who == 1 and last:
                            continue
                        gi = (0 if j < 6 else 2) + who
                        gt = (gx1, gc1, gx2, gc2)[gi]
                        bank = 1 + (half * 2 + who)
                        for kc in range(8):
                            PE(lambda h, wj=wj, kc=kc, who=who, bank=bank: h.matmul(PS[:, bank, :], lhsT=scB[:, who, kc, :], rhs=wj[:, kc, :],
                                                                                    start=(kc == 0), stop=(kc == 7)),
                               [dwj, d_scB], [], [d_ps[bank]])
                        V(lambda h, gt=gt, bank=bank, half=half: h.tensor_tensor(out=gt[:, half * 512:(half + 1) * 512], in0=PS[:, bank, :],
                                                                                 in1=gt[:, half * 512:(half + 1) * 512], op=ALU.add),
                          [d_gates[gi]], [d_gates[gi]], [d_ps[bank]])
            V(lambda h: h.tensor_tensor(out=modfm, in0=psfm.rearrange("p (c j) -> p c j", j=2), in1=bfm[:, :, None].to_broadcast([128, 48, 2]),
                                        op=ALU.add), [d_bfm], [d_mod], [d_ps[0]])
            V(lambda h: h.scalar_tensor_tensor(out=scale1, in0=modfm[:, 8:16, :], scalar=1.0,
                                               in1=ng[:, l, 0, :, None].to_broadcast([128, 8, 2]), op0=ALU.add, op1=ALU.mult),
              [d_mod, d_ng], [d_mod])
            V(lambda h: h.scalar_tensor_tensor(out=scale2, in0=modfm[:, 32:40, :], scalar=1.0,
                                               in1=ng[:, l, 1, :, None].to_broadcast([128, 8, 2]), op0=ALU.add, op1=ALU.mult),
              [d_mod, d_ng], [d_mod])
            S.dma("sp", lambda h: h.dma_start(out=lamt_flat, in_=lamv_d[l].partition_broadcast(128)), writes=[d_lam])
            S.dma("sp", lambda h: h.dma_start(out=gA, in_=subln_d[l].partition_broadcast(128)), writes=[d_gA])
            S.dma("sp", lambda h: h.dma_start(out=sinkt, in_=sink_d[l].partition_broadcast(128)), writes=[d_sink])
            for i in range(2):
                V(lambda h, i=i: h.tensor_tensor(out=lamj, in0=lamt[:, 2 * i, :], in1=lamt[:, 2 * i + 1, :], op=ALU.mult), [d_lam], [d_lam])
                V(lambda h, i=i: h.tensor_reduce(out=lams[:, i:i + 1], in_=lamj, axis=AX.X, op=ALU.add), [d_lam], [d_lam])
            A(lambda h: h.activation(out=lams, in_=lams, func=AF.Exp), [d_lam], [d_lam])
            V(lambda h: h.scalar_tensor_tensor(out=neglam, in0=lams[:, 1:2], scalar=-lambda_init, in1=lams[:, 0:1], op0=ALU.add, op1=ALU.subtract),
              [d_lam], [d_lam])
            V(lambda h: h.tensor_scalar(out=gA, in0=gA, scalar1=(1.0 - lambda_init), scalar2=None, op0=ALU.mult), [d_gA], [d_gA])
            A(lambda h: h.activation(out=esink, in_=sinkt, func=AF.Exp), [d_sink], [d_sink])
            S.barrier()
            if debug and l == 0:
                dump("modfm", modfm, [d_mod])
                dump("gx1", gx1, [d_gates[0]])
                dump("gc2", gc2, [d_gates[3]])

            groups = [[0, 1, 2, 3], [4, 5, 6, 7], [8, 9, 10, 11], [12, 13, 14, 15], [16, 17]]
            if l == 0:
                def loader(t, dst_ap, dst_dep):
                    srcd = x_d[t * 128:(t + 1) * 128, :] if t < NTL else ctx_d[(t - NTL) * 128:(t - NTL + 1) * 128, :]
                    S.dma("sp", lambda h, srcd=srcd, dst_ap=dst_ap: h.dma_start(out=dst_ap, in_=srcd), writes=[dst_dep])

                rms_to_featmajor(None, groups, scale1, 0, hT, d_hT, None, loader=loader)
            else:
                rms_to_featmajor(lambda t: xres[:, t, :], groups, scale1, 0, hT, d_hT, lambda t: d_xres[t])
                for q4 in range(4):
                    S.dma("sp", lambda h, q4=q4: h.dma_start(out=xsp_d[q4 * 512:(q4 + 1) * 512, :].rearrange("(t p) d -> p t d", p=128),
                                                             in_=xres[:, q4 * 4:(q4 + 1) * 4, :]),
                          reads=d_xres[q4 * 4:(q4 + 1) * 4], writes=[d_xsp])
            S.barrier()
            if debug and l == 0:
                dump("hT", hT, [d_hT], BF16)
            if stop_after == "p1":
                return True

            RR1.reset()
            qk = RR1.alloc([128, 14, T], BF16)
            vt = RR1.alloc([128, NT, 784], BF16)
            RL.reset()
            ropeC = RL.alloc([128, SEQ], BF16)
            ropeS = RL.alloc([128, SEQ], BF16)
            wq = [RL.alloc([128, 8, 128], BF16) for _ in range(3)]
            wr = [RL.alloc([128, 8, 128], BF16) for _ in range(3)]
            wv = RL.alloc([128, 8, 768], BF16)
            t1 = [RL.alloc([128, 512], F32) for _ in range(2)]
            t2 = [RL.alloc([128, 512], F32) for _ in range(2)]
            sqb = [RL.alloc([128, 512], F32) for _ in range(2)]
            rsb = [RL.alloc([128, 512], F32) for _ in range(2)]
            d_rope = Dep("rope")
            d_wq = [Dep("wq%d" % i) for i in range(3)]
            d_wr = [Dep("wr%d" % i) for i in range(3)]
            d_wv = Dep("wv")
            d_t1 = [Dep("t1%d" % i) for i in range(2)]
            d_t2 = [Dep("t2%d" % i) for i in range(2)]
            d_sq = [Dep("sq%d" % i) for i in range(2)]
            d_rs = [Dep("rs%d" % i) for i in range(2)]
            d_qk = [Dep("qk%d" % i) for i in range(14)]
            d_v = [Dep("v%d" % i) for i in range(NT)]
            d_vones = Dep("vones")
            S.dma("pool", lambda h: h.dma_start(out=ropeC, in_=ropeC_d), writes=[d_rope])
            S.dma("pool", lambda h: h.dma_start(out=ropeS, in_=ropeS_d), writes=[d_rope])
            S.dma("pool", lambda h: h.dma_start(out=wv, in_=w_in_d[l, :, 1792:2560].rearrange("(kc p) n -> p kc n", p=128)), writes=[d_wv])
            for t in range(NT):
                G(lambda h, t=t: h.memset(vt[:, t, :], 1.0), [], [d_v[t]])
            it = 0
            wst = [RL.alloc([128, 8, 128], F32) for _ in range(2)]
            d_wst = [Dep("wst0"), Dep("wst1")]

            def load_w(ci):
                sl = ci % 3
                ss = ci % 2
                S.dma("sp", lambda h: h.dma_start(out=wst[ss], in_=w_in_d[l, :, ci * 128:(ci + 1) * 128].rearrange("(kc p) n -> p kc n", p=128)),
                      writes=[d_wst[ss]])
                ws5 = wst[ss].rearrange("p k (u two s) -> p k u two s", two=2, s=16)
                wr5 = wr[sl].rearrange("p k (u two s) -> p k u two s", two=2, s=16)
                A(lambda h: h.activation(out=wq[sl], in_=wst[ss], func=AF.Copy), [d_wst[ss]], [d_wq[sl]])
                for kh in range(2):
                    A(lambda h, kh=kh: h.activation(out=wr5[:, kh * 4:(kh + 1) * 4, :, 0, :], in_=ws5[:, kh * 4:(kh + 1) * 4, :, 1, :], func=AF.Copy, scale=-1.0),
                      [d_wst[ss]], [d_wr[sl]])
                    A(lambda h, kh=kh: h.activation(out=wr5[:, kh * 4:(kh + 1) * 4, :, 1, :], in_=ws5[:, kh * 4:(kh + 1) * 4, :, 0, :], func=AF.Copy),
                      [d_wst[ss]], [d_wr[sl]])

            load_w(0)
            load_w(1)
            for ci in range(14):
                sl = ci % 3
                if ci + 2 < 14:
                    load_w(ci + 2)
                isB = ci in (4, 5, 12)
                gcol = 0 if ci < 8 else 2
                for n in range(4):
                    pa = (it % 2) * 2
                    pb = pa + 1
                    tb = it % 2
                    it += 1
                    cs = slice(n * 512, (n + 1) * 512)
                    for kc in range(8):
                        PE(lambda h, pa=pa, sl=sl, kc=kc, cs=cs: h.matmul(PS[:, pa, :], lhsT=wq[sl][:, kc, :], rhs=hT[:, kc, cs], start=(kc == 0), stop=(kc == 7)),
                           [d_wq[sl], d_hT], [], [d_ps[pa]])
                    for kc in range(8):
                        PE(lambda h, pb=pb, sl=sl, kc=kc, cs=cs: h.matmul(PS[:, pb, :], lhsT=wr[sl][:, kc, :], rhs=hT[:, kc, cs], start=(kc == 0), stop=(kc == 7)),
                           [d_wr[sl], d_hT], [], [d_ps[pb]])
                    if not isB:
                        V(lambda h, pa=pa, tb=tb, cs=cs: h.tensor_tensor(out=t1[tb], in0=PS[:, pa, :], in1=ropeC[:, cs], op=ALU.mult),
                          [d_rope], [d_t1[tb]], [d_ps[pa]])
                        V(lambda h, pb=pb, tb=tb, cs=cs: h.tensor_tensor(out=t2[tb], in0=PS[:, pb, :], in1=ropeS[:, cs], op=ALU.mult),
                          [d_rope], [d_t2[tb]], [d_ps[pb]])
                        G(lambda h, tb=tb, ci=ci, cs=cs: h.tensor_tensor(out=qk[:, ci, cs], in0=t1[tb], in1=t2[tb], op=ALU.add),
                          [d_t1[tb], d_t2[tb]], [d_qk[ci]])
                    else:
                        A(lambda h, pa=pa, tb=tb: h.activation(out=sqb[tb], in_=PS[:, pa, :], func=AF.Square), [], [d_sq[tb]], [d_ps[pa]])
                        pm = 4 + tb
                        PE(lambda h, pm=pm, tb=tb: h.matmul(PS[:, pm, :], lhsT=blk64, rhs=sqb[tb], start=True, stop=True),
                           [d_sq[tb], d_const], [], [d_ps[pm]])
                        A(lambda h, pm=pm, tb=tb: h.activation(out=rsb[tb], in_=PS[:, pm, :], func=AF.Sqrt, bias=epsD[:, 0:1]),
                          [d_const], [d_rs[tb]], [d_ps[pm]])
                        V(lambda h, tb=tb: h.reciprocal(out=rsb[tb], in_=rsb[tb]), [d_rs[tb]], [d_rs[tb]])
                        V(lambda h, pa=pa, tb=tb, cs=cs, gcol=gcol: h.scalar_tensor_tensor(out=t1[tb], in0=PS[:, pa, :], scalar=qkn[:, l, gcol:gcol + 1],
                                                                                          in1=ropeC[:, cs], op0=ALU.mult, op1=ALU.mult),
                          [d_rope, d_qkn], [d_t1[tb]], [d_ps[pa]])
                        V(lambda h, pb=pb, tb=tb, cs=cs, gcol=gcol: h.scalar_tensor_tensor(out=t2[tb], in0=PS[:, pb, :], scalar=qkn[:, l, gcol + 1:gcol + 2],
                                                                                          in1=ropeS[:, cs], op0=ALU.mult, op1=ALU.mult),
                          [d_rope, d_qkn], [d_t2[tb]], [d_ps[pb]])
                        G(lambda h, tb=tb: h.tensor_tensor(out=t1[tb], in0=t1[tb], in1=t2[tb], op=ALU.add), [d_t1[tb], d_t2[tb]], [d_t1[tb]])
                        G(lambda h, tb=tb, ci=ci, cs=cs: h.tensor_tensor(out=qk[:, ci, cs], in0=t1[tb], in1=rsb[tb], op=ALU.mult),
                          [d_t1[tb], d_rs[tb]], [d_qk[ci]])
                if ci >= 8 or not last:
                    pa = (it % 2) * 2
                    tb = it % 2
                    it += 1
                    cs = slice(SEQ, T)
                    for kc in range(8):
                        PE(lambda h, pa=pa, sl=sl, kc=kc, cs=cs: h.matmul(PS[:, pa, 0:CTXL], lhsT=wq[sl][:, kc, :], rhs=hT[:, kc, cs], start=(kc == 0), stop=(kc == 7)),
                           [d_wq[sl], d_hT], [], [d_ps[pa]])
                    if not isB:
                        A(lambda h, pa=pa, ci=ci, cs=cs: h.activation(out=qk[:, ci, cs], in_=PS[:, pa, 0:CTXL], func=AF.Copy), [], [d_qk[ci]], [d_ps[pa]])
                    else:
                        A(lambda h, pa=pa, tb=tb: h.activation(out=sqb[tb][:, 0:CTXL], in_=PS[:, pa, 0:CTXL], func=AF.Square), [], [d_sq[tb]], [d_ps[pa]])
                        pm = 4 + tb
                        PE(lambda h, pm=pm, tb=tb: h.matmul(PS[:, pm, 0:CTXL], lhsT=blk64, rhs=sqb[tb][:, 0:CTXL], start=True, stop=True),
                           [d_sq[tb], d_const], [], [d_ps[pm]])
                        A(lambda h, pm=pm, tb=tb: h.activation(out=rsb[tb][:, 0:CTXL], in_=PS[:, pm, 0:CTXL], func=AF.Sqrt, bias=epsD[:, 0:1]),
                          [d_const], [d_rs[tb]], [d_ps[pm]])
                        V(lambda h, tb=tb: h.reciprocal(out=rsb[tb][:, 0:CTXL], in_=rsb[tb][:, 0:CTXL]), [d_rs[tb]], [d_rs[tb]])
                        V(lambda h, pa=pa, tb=tb, ci=ci, cs=cs, gcol=gcol: h.scalar_tensor_tensor(out=qk[:, ci, cs], in0=PS[:, pa, 0:CTXL],
                                                                                                 scalar=qkn[:, l, gcol:gcol + 1], in1=rsb[tb][:, 0:CTXL],
                                                                                                 op0=ALU.mult, op1=ALU.mult),
                          [d_rs[tb], d_qkn], [d_qk[ci]], [d_ps[pa]])
            for t in range(NT):
                b1 = 6 + (t % 2)
                ts_ = slice(t * 128, (t + 1) * 128)
                for kc in range(8):
                    PE(lambda h, b1=b1, kc=kc, ts_=ts_: h.matmul(PS[:, b1, :], lhsT=hT[:, kc, ts_], rhs=wv[:, kc, 0:512], start=(kc == 0), stop=(kc == 7)),
                       [d_hT, d_wv], [], [d_ps[b1]])
                A(lambda h, b1=b1, t=t: h.activation(out=vt[:, t, 0:520].rearrange("p (h c) -> p h c", c=130)[:, :, 0:128],
                                                     in_=PS[:, b1, :].rearrange("p (h c) -> p h c", c=128), func=AF.Copy),
                  [], [d_v[t]], [d_ps[b1]])
                b2 = 4 + (t % 2)
                for kc in range(8):
                    PE(lambda h, b2=b2, kc=kc, ts_=ts_: h.matmul(PS[:, b2, 0:256], lhsT=hT[:, kc, ts_], rhs=wv[:, kc, 512:768], start=(kc == 0), stop=(kc == 7)),
                       [d_hT, d_wv], [], [d_ps[b2]])
                V(lambda h, b2=b2, t=t: h.tensor_copy(out=vt[:, t, 520:784].rearrange("p (h c) -> p h c", c=66)[:, :, 0:64],
                                                      in_=PS[:, b2, 0:256].rearrange("p (h c) -> p h c", c=64)),
                  [], [d_v[t]], [d_ps[b2]])
            S.barrier()
            if debug and l == 0:
                dump("qk", qk, d_qk, BF16)
                dump("vt", vt, d_v, BF16)
            if stop_after == "p2":
                return True

            mixT = hT
            d_mix = Dep("mixT")
            RL.reset()
            wo = RL.alloc([128, 8, D], BF16)
            d_wo = Dep("wo")
            S.dma("pool", lambda h: h.dma_start(out=wo, in_=w_out_d[l].rearrange("(kc p) n -> p kc n", p=128)), writes=[d_wo])
            Pb = [RL.alloc([128, 2, 512], BF16) for _ in range(3)]
            d_P = [Dep("P%d" % i) for i in range(3)]
            accs_sb = [RL.alloc([128, 3, 512], F32) for _ in range(3)]
            d_accs = [[Dep("accs%d_%d" % (u, k)) for k in range(3)] for u in range(3)]
            eo = [RL.alloc([128, 4, 128], F32) for _ in range(3)]
            et = [RL.alloc([128, 128], F32) for _ in range(2)]
            eon = [RL.alloc([128, 4, 128], BF16) for _ in range(3)]
            ejunk = RL.alloc([128, 128], BF16)
            esm = [RL.alloc([128, 4, 4], F32) for _ in range(3)]
            d_eo = [Dep("eo%d" % i) for i in range(3)]
            d_et = [Dep("et%d" % i) for i in range(2)]
            d_eon = [Dep("eon%d" % i) for i in range(3)]
            d_ej = Dep("ejunk")
            d_esm = [Dep("esm%d" % i) for i in range(3)]
            qz = [RL.alloc([128, 2, 512], BF16) for _ in range(3)]
            d_qz = [Dep("qz0"), Dep("qz1"), Dep("qz2")]
            state = {"sb": 0, "pb": 0, "ep": 0, "u": 0, "zq": 0}

            units = []

            def attn_unit(kind, streams, nq, q0, ktiles, vcol, dv, masks, mix_chunk, sink_cols=None, valid=None):
                units.append(dict(kind=kind, streams=streams, nq=nq, q0=q0, ktiles=ktiles, vcol=vcol, dv=dv, masks=masks,
                                  mix_chunk=mix_chunk, sink_cols=sink_cols, valid=valid))

            def prep_qz(U, zq):
                qzu = qz[zq]
                nq, q0 = U["nq"], U["q0"]
                G(lambda h: h.memset(qzu[:, :, 0:nq], 0.0), [], [d_qz[zq]])
                for s_, (qc, kc_, base) in enumerate(U["streams"]):
                    G(lambda h, s_=s_, qc=qc, base=base: h.tensor_copy(out=qzu[base:base + 64, s_, 0:nq], in_=qk[base:base + 64, qc, q0:q0 + nq]),
                      [d_qk[qc]], [d_qz[zq]])

            class UnitCtx:
                def __init__(self, U, ui):
                    self.U = U
                    self.ui = ui
                    self.zq = ui % 3
                    self.u = ui % 3
                    nq, dv = U["nq"], U["dv"]
                    self.nj = nq // 128
                    self.stride = 130 if dv == 128 else 66
                    self.perbank = 3 if dv == 128 else 4
                    self.accs = []
                    for s_ in range(2):
                        for j in range(self.nj):
                            a = s_ * self.nj + j
                            self.accs.append((a // self.perbank, (a % self.perbank) * self.stride, a))
                    self.nbanks = self.accs[-1][0] + 1
                    self.started = set()

            def emit_s_exp(C, ki):
                U = C.U
                nq, kt = U["nq"], U["ktiles"][ki]
                qzu = qz[C.zq]
                sb = state["sb"]
                state["sb"] ^= 1
                pbi = state["pb"]
                state["pb"] = (pbi + 1) % 3
                ks = slice(kt * 128, (kt + 1) * 128)
                for s_, (qc, kc_, base) in enumerate(U["streams"]):
                    PE(lambda h, s_=s_, kc_=kc_: h.matmul(PS[:, sb * 2 + s_, 0:nq], lhsT=qk[:, kc_, ks], rhs=qzu[:, s_, 0:nq], start=True, stop=True),
                       [d_qz[C.zq], d_qk[kc_]], [], [d_ps[sb * 2 + s_]])
                    for (mkt, mj), mk in U["masks"].items():
                        if mkt != kt:
                            continue
                        PE(lambda h, s_=s_, mk=mk, mj=mj: h.matmul(PS[:, sb * 2 + s_, mj * 128:(mj + 1) * 128], lhsT=identb, rhs=mk, start=False, stop=True,
                                                                  skip_group_check=True),
                           [d_const], [], [d_ps[sb * 2 + s_]])
                A(lambda h: h.activation(out=Pb[pbi][:, :, 0:nq], in_=PS[:, sb * 2:sb * 2 + 2, 0:nq], func=AF.Exp, scale=0.125),
                  [], [d_P[pbi]], [d_ps[sb * 2], d_ps[sb * 2 + 1]])
                return pbi

            def emit_pv(C, ki, pbi):
                U = C.U
                kt, dv, vcol, valid, nj = U["ktiles"][ki], U["dv"], U["vcol"], U["valid"], C.nj
                for (bk, off, a) in C.accs:
                    s_ = a // nj
                    j = a % nj
                    if valid is not None and j not in valid[kt]:
                        continue
                    st_flag = bk not in C.started
                    C.started.add(bk)
                    PE(lambda h, bk=bk, off=off, s_=s_, j=j, st_flag=st_flag: h.matmul(
                        PS[:, 4 + bk, off:off + dv + 1], lhsT=Pb[pbi][:, s_, j * 128:(j + 1) * 128], rhs=vt[:, kt, vcol:vcol + dv + 1],
                        start=st_flag, stop=False, skip_group_check=True),
                       [d_P[pbi], d_v[kt]], [], [d_ps[4 + bk]])

            def emit_copyout(C):
                u = C.u
                asb = accs_sb[u]
                for bk in range(C.nbanks):
                    ncols = min(C.perbank, len(C.accs) - bk * C.perbank) * C.stride
                    V(lambda h, bk=bk, ncols=ncols: h.tensor_copy(out=asb[:, bk, 0:ncols], in_=PS[:, 4 + bk, 0:ncols]),
                      [], [d_accs[u][bk]], [d_ps[4 + bk]])

            def make_stages(C):
                U = C.U
                kind, nq, q0, dv, mix_chunk, sink_cols = U["kind"], U["nq"], U["q0"], U["dv"], U["mix_chunk"], U["sink_cols"]
                u, nj, accs = C.u, C.nj, C.accs
                asb = accs_sb[u]
                sm = esm[u]
                dsm = d_esm[u]
                eou = eo[u]
                deo = d_eo[u]
                eonu = eon[u]
                deon = d_eon[u]

                def parts(j):
                    a0 = accs[j]
                    a1 = accs[nj + j]
                    return (asb[:, a0[0], a0[1] + dv:a0[1] + dv + 1], asb[:, a1[0], a1[1] + dv:a1[1] + dv + 1],
                            asb[:, a0[0], a0[1]:a0[1] + dv], asb[:, a1[0], a1[1]:a1[1] + dv], d_accs[u][a0[0]], d_accs[u][a1[0]])

                def stage1():
                    if kind == "A":
                        for j in range(nj):
                            z0, z1, o0, o1, da0, da1 = parts(j)
                            V(lambda h, z0=z0, j=j: h.reciprocal(out=sm[:, 0, j:j + 1], in_=z0), [da0], [dsm])
                            V(lambda h, z1=z1, j=j: h.reciprocal(out=sm[:, 1, j:j + 1], in_=z1), [da1], [dsm])
                            V(lambda h, j=j: h.tensor_tensor(out=sm[:, 1, j:j + 1], in0=sm[:, 1, j:j + 1], in1=neglam, op=ALU.mult), [dsm, d_lam], [dsm])
                            V(lambda h, o1=o1, j=j: h.tensor_scalar(out=et[0], in0=o1, scalar1=sm[:, 1, j:j + 1], scalar2=None, op0=ALU.mult),
                              [da1, dsm], [d_et[0]])
                            V(lambda h, o0=o0, j=j: h.scalar_tensor_tensor(out=eou[:, j, :], in0=o0, scalar=sm[:, 0, j:j + 1], in1=et[0], op0=ALU.mult, op1=ALU.add),
                              [da0, dsm, d_et[0]], [deo])
                            V(lambda h, j=j: h.scalar_tensor_tensor(out=et[1], in0=eou[:, j, :], scalar=1.0, in1=eou[:, j, :], op0=ALU.mult, op1=ALU.mult,
                                                                    accum_out=sm[:, 2, j:j + 1]), [deo], [d_et[1], dsm])
                        V(lambda h: h.tensor_scalar(out=sm[:, 3, 0:nj], in0=sm[:, 2, 0:nj], scalar1=1.0 / 128, scalar2=RMS_EPS, op0=ALU.mult, op1=ALU.add),
                          [dsm], [dsm])
                        G(lambda h: h.tensor_tensor(out=sm[:, 3, 0:nj], in0=sm[:, 3, 0:nj], in1=mhalf[:, 0:nj], op=ALU.pow), [dsm, d_const], [dsm])
                        for j in range(nj):
                            V(lambda h, j=j: h.scalar_tensor_tensor(out=eonu[:, j, :], in0=eou[:, j, :], scalar=sm[:, 3, j:j + 1], in1=gA, op0=ALU.mult, op1=ALU.mult),
                              [deo, dsm, d_gA], [deon])
                    else:
                        if nj == 4:
                            av = asb[:, 0:2, 0:264].rearrange("p s (j c) -> p s j c", c=66)
                        else:
                            av = asb[:, 0, 0:264].rearrange("p (s j c) -> p s j c", s=2, j=2)
                        zv = av[:, :, :, 64]
                        ov = av[:, :, :, 0:64]
                        rz = sm[:, 0:2, 0:nj]
                        dacc = [d_accs[u][0], d_accs[u][1]] if nj == 4 else [d_accs[u][0]]
                        if sink_cols is not None:
                            V(lambda h: h.tensor_tensor(out=rz, in0=zv, in1=esink[:, sink_cols[0]:sink_cols[0] + 2, None].to_broadcast([128, 2, nj]), op=ALU.add),
                              dacc + [d_sink], [dsm])
                            V(lambda h: h.reciprocal(out=rz, in_=rz), [dsm], [dsm])
                        else:
                            V(lambda h: h.reciprocal(out=rz, in_=zv), dacc, [dsm])
                        V(lambda h: h.tensor_tensor(out=eonu[:, 0:nj, :].rearrange("p j (s c) -> p j s c", s=2),
                                                    in0=ov.rearrange("p s j c -> p j s c"),
                                                    in1=rz.rearrange("p s j -> p j s")[:, :, :, None].to_broadcast([128, nj, 2, 64]), op=ALU.mult),
                          dacc + [dsm], [deon])

                def stage2():
                    tpv = PS[:, 7, :].bitcast(BF16)[:, 0:nj * 128]
                    for j in range(nj):
                        PE(lambda h, j=j: h.transpose(out=tpv[:, j * 128:(j + 1) * 128], in_=eonu[:, j, :], identity=identb), [deon, d_const], [], [d_ps[7]])
                    V(lambda h: h.tensor_copy(out=mixT[:, mix_chunk, q0:q0 + nq], in_=tpv), [], [d_mix], [d_ps[7]])

                return stage1, stage2

            allk = list(range(NT))
            ctxk = [16, 17]
            for hd in range(4):
                streams = [(hd, 8 + hd, 0), (hd, 8 + hd, 64)]
                for qb in range(4):
                    attn_unit("A", streams, 512, qb * 512, allk, hd * 130, 128, {}, hd)
                if not last:
                    attn_unit("A", streams, 256, SEQ, ctxk, hd * 130, 128, {}, hd)
            for j in range(2):
                streams = [(4, 12, 64 * j), (5, 12, 64 * j)]
                for qb in range(4):
                    attn_unit("B", streams, 512, qb * 512, allk, 520 + j * 66, 64, {}, 4 + j)
                if not last:
                    attn_unit("B", streams, 256, SEQ, ctxk, 520 + j * 66, 64, {}, 4 + j)
            for j in range(2):
                streams = [(6, 13, 64 * j), (7, 13, 64 * j)]
                sc_ = (2 * j, 2 * j + 1)
                for qb in range(4):
                    kts = list(ctxk)
                    valid = {16: [0, 1, 2, 3], 17: [0, 1, 2, 3]}
                    masks = {}
                    for t in range(4 * qb - 1, 4 * qb + 5):
                        if t < 0 or t >= NTL:
                            continue
                        kts.append(t)
                        valid[t] = [jj for jj in range(4) if abs(t - (4 * qb + jj)) <= 1]
                        for jj in valid[t]:
                            qt = 4 * qb + jj
                            if t == qt - 1:
                                masks[(t, jj)] = mprevn
                            elif t == qt + 1:
                                masks[(t, jj)] = mnextn
                    attn_unit("C", streams, 512, qb * 512, kts, 652 + j * 66, 64, masks, 6 + j, sink_cols=sc_, valid=valid)
                if not last:
                    attn_unit("C", streams, 256, SEQ, ctxk, 652 + j * 66, 64, {}, 6 + j, sink_cols=sc_)
            its = [(ui, ki) for ui, U in enumerate(units) for ki in range(len(U["ktiles"]))]
            ctxs = {}
            pending = []

            def flush(owner_le=None, g=None):
                keep = []
                for ent in pending:
                    if (owner_le is not None and ent[2] <= owner_le) or (g is not None and ent[0] <= g):
                        ent[1]()
                    else:
                        keep.append(ent)
                pending[:] = keep

            prep_qz(units[0], 0)
            if len(units) > 1:
                prep_qz(units[1], 1)
            pvq = []

            def pop_pv(g):
                Cp, kip, pbip = pvq.pop(0)
                emit_pv(Cp, kip, pbip)
                if kip == len(Cp.U["ktiles"]) - 1:
                    flush(owner_le=Cp.ui - 3)
                    emit_copyout(Cp)
                    s1, s2 = make_stages(Cp)
                    pending.append([g + 2, s1, Cp.ui])
                    pending.append([g + 16, s2, Cp.ui])

            for g, (ui, ki) in enumerate(its):
                if ui not in ctxs:
                    ctxs[ui] = UnitCtx(units[ui], ui)
                C = ctxs[ui]
                pbi = emit_s_exp(C, ki)
                pvq.append((C, ki, pbi))
                if ki == 0 and g > 0:
                    while len(pvq) > 1:
                        pop_pv(g)
                elif len(pvq) > 2:
                    pop_pv(g)
                n = len(C.U["ktiles"])
                if ki == min(4, n - 1) and ui + 2 < len(units):
                    prep_qz(units[ui + 2], (ui + 2) % 3)
                flush(g=g)
            g = len(its)
            while pvq:
                pop_pv(g)
            flush(owner_le=len(units))
            S.barrier()
            if debug and l == 0:
                dump("mixT", mixT, [d_mix], BF16)
            if stop_after == "p3":
                return True

            RR1.reset()
            xres = RR1.alloc([128, NT, D], F32)
            GS['xres'] = xres
            RR1x = Region(SB, RR1.base + RR1.off, R1_SIZE - RR1.off)
            RL.off = 8 * D * 2
            tmpo = [RL.alloc([128, D], F32) for _ in range(2)]
            d_tmpo = [Dep("tmpo0"), Dep("tmpo1")]
            for t in tok_tiles:
                if l == 0:
                    srcd = x_d[t * 128:(t + 1) * 128, :] if t < NTL else ctx_d[(t - NTL) * 128:(t - NTL + 1) * 128, :]
                    S.dma("sp", lambda h, t=t, srcd=srcd: h.dma_start(out=xres[:, t, :], in_=srcd), writes=[d_xres[t]])
                else:
                    S.dma("sp", lambda h, t=t: h.dma_start(out=xres[:, t, :], in_=xsp_d[t * 128:(t + 1) * 128, :]), reads=[d_xsp], writes=[d_xres[t]])
            for ti, t in enumerate(tok_tiles):
                pb0 = (ti % 2) * 2
                gt = gx1 if t < NTL else gc1
                gd = d_gates[0] if t < NTL else d_gates[1]
                ts_ = slice(t * 128, (t + 1) * 128)
                for half in range(2):
                    for kc in range(8):
                        PE(lambda h, pb0=pb0, half=half, kc=kc, ts_=ts_: h.matmul(PS[:, pb0 + half, :], lhsT=mixT[:, kc, ts_], rhs=wo[:, kc, half * 512:(half + 1) * 512],
                                                                                  start=(kc == 0), stop=(kc == 7)),
                           [d_mix, d_wo], [], [d_ps[pb0 + half]])
                tb = ti % 2
                V(lambda h, pb0=pb0, tb=tb, gt=gt: h.tensor_tensor(out=tmpo[tb], in0=PS[:, pb0:pb0 + 2, :].rearrange("p a b -> p (a b)"), in1=gt, op=ALU.mult),
                  [gd], [d_tmpo[tb]], [d_ps[pb0], d_ps[pb0 + 1]])
                G(lambda h, tb=tb, t=t: h.tensor_tensor(out=xres[:, t, :], in0=xres[:, t, :], in1=tmpo[tb], op=ALU.add),
                  [d_tmpo[tb], d_xres[t]], [d_xres[t]])
            S.barrier()
            if debug and l == 0:
                dump("xres_attn", xres, d_xres)
            if stop_after == "p4":
                return True

            h2T = hT
            d_h2 = Dep("h2T")
            groups2 = [[0, 1, 2, 3], [4, 5, 6, 7], [8, 9, 10, 11], [12, 13, 14, 15]] + ([] if last else [[16, 17]])
            rms_to_featmajor(lambda t: xres[:, t, :], groups2, scale2, 24, h2T, d_h2, lambda t: d_xres[t])
            S.barrier()
            if debug and l == 0:
                dump("h2T", h2T, [d_h2], BF16)

            RL.reset()
            ntt = len(tok_tiles)
            scr = RL.alloc([128, NT, NE], F32)
            sel = RL.alloc([128, NT, NE], F32)
            ta = RL.alloc([128, NT * 4, 3], F32)
            tb_ = RL.alloc([128, NT * 4, 2], F32)
            tc = RL.alloc([128, NT * 4], F32)
            gs = RL.alloc([128, NT * 4], F32)
            gs2 = RL.alloc([128, NT * 4], F32)
            gmax = RL.alloc([128, NT], F32)
            gsel = RL.alloc([128, NT, 4], F32)
            m1 = RL.alloc([128, NT * 4], F32)
            is1 = RL.alloc([128, NT * 4, 4], F32)
            selm = RL.alloc([128, NT * 4, 4], F32)
            m2 = RL.alloc([128, NT * 4], F32)
            top2 = RL.alloc([128, NT * 4, 4], F32)
            msk = RL.alloc([128, NT, 4, 4], F32)
            wgt = RL.alloc([128, NT, NE], F32)
            wsum = RL.alloc([128, NT], F32)
            gates = RL.alloc([128, NT, NE], F32)
            d_r = Dep("router")
            d_gt = Dep("gates")
            rps = PS[:, 0, 0:NT * NE].rearrange("p (t e) -> p t e", e=NE)
            for t in tok_tiles:
                for kc in range(8):
                    PE(lambda h, t=t, kc=kc: h.matmul(rps[:, t, :], lhsT=h2T[:, kc, t * 128:(t + 1) * 128], rhs=wrt[:, kc, :], start=(kc == 0), stop=(kc == 7)),
                       [d_h2, d_wrt], [], [d_ps[0]])
            A(lambda h: h.activation(out=scr[:, 0:ntt, :], in_=rps[:, 0:ntt, :], func=AF.Sigmoid), [], [d_r], [d_ps[0]])
            n_ = ntt
            sel4 = sel[:, 0:n_, :].rearrange("p t (g e) -> p (t g) e", g=4)
            R_ = lambda fn: V(fn, [d_r, d_rb], [d_r])
            R_(lambda h: h.tensor_tensor(out=sel[:, 0:n_, :], in0=scr[:, 0:n_, :], in1=rbias[:, None, :].to_broadcast([128, n_, NE]), op=ALU.add))
            R_(lambda h: h.tensor_tensor(out=ta[:, 0:n_ * 4, :], in0=sel4[:, :, 0:3], in1=sel4[:, :, 1:4], op=ALU.add))
            R_(lambda h: h.tensor_tensor(out=tb_[:, 0:n_ * 4, :], in0=sel4[:, :, 0:2], in1=sel4[:, :, 2:4], op=ALU.add))
            R_(lambda h: h.tensor_tensor(out=tc[:, 0:n_ * 4], in0=sel4[:, :, 0], in1=sel4[:, :, 3], op=ALU.add))
            R_(lambda h: h.tensor_reduce(out=gs[:, 0:n_ * 4], in_=ta[:, 0:n_ * 4, :], axis=AX.X, op=ALU.max))
            R_(lambda h: h.tensor_reduce(out=gs2[:, 0:n_ * 4], in_=tb_[:, 0:n_ * 4, :], axis=AX.X, op=ALU.max))
            R_(lambda h: h.tensor_tensor(out=gs[:, 0:n_ * 4], in0=gs[:, 0:n_ * 4], in1=gs2[:, 0:n_ * 4], op=ALU.max))
            R_(lambda h: h.tensor_tensor(out=gs[:, 0:n_ * 4], in0=gs[:, 0:n_ * 4], in1=tc[:, 0:n_ * 4], op=ALU.max))
            gs3 = gs[:, 0:n_ * 4].rearrange("p (t g) -> p t g", g=4)
            R_(lambda h: h.tensor_reduce(out=gmax[:, 0:n_], in_=gs3, axis=AX.X, op=ALU.max))
            R_(lambda h: h.tensor_tensor(out=gsel[:, 0:n_, :], in0=gs3, in1=gmax[:, 0:n_, None].to_broadcast([128, n_, 4]), op=ALU.is_ge))
            R_(lambda h: h.tensor_reduce(out=m1[:, 0:n_ * 4], in_=sel4, axis=AX.X, op=ALU.max))
            R_(lambda h: h.tensor_tensor(out=is1[:, 0:n_ * 4, :], in0=sel4, in1=m1[:, 0:n_ * 4, None].to_broadcast([128, n_ * 4, 4]), op=ALU.is_ge))
            R_(lambda h: h.scalar_tensor_tensor(out=selm[:, 0:n_ * 4, :], in0=is1[:, 0:n_ * 4, :], scalar=-1e9, in1=sel4, op0=ALU.mult, op1=ALU.add))
            R_(lambda h: h.tensor_reduce(out=m2[:, 0:n_ * 4], in_=selm[:, 0:n_ * 4, :], axis=AX.X, op=ALU.max))
            R_(lambda h: h.tensor_tensor(out=top2[:, 0:n_ * 4, :], in0=sel4, in1=m2[:, 0:n_ * 4, None].to_broadcast([128, n_ * 4, 4]), op=ALU.is_ge))
            top2v = top2[:, 0:n_ * 4, :].rearrange("p (t g) e -> p t g e", g=4)
            R_(lambda h: h.tensor_tensor(out=msk[:, 0:n_], in0=top2v, in1=gsel[:, 0:n_, :, None].to_broadcast([128, n_, 4, 4]), op=ALU.mult))
            R_(lambda h: h.tensor_tensor(out=wgt[:, 0:n_, :], in0=scr[:, 0:n_, :], in1=msk[:, 0:n_].rearrange("p t g e -> p t (g e)"), op=ALU.mult))
            R_(lambda h: h.tensor_reduce(out=wsum[:, 0:n_], in_=wgt[:, 0:n_, :], axis=AX.X, op=ALU.add))
            R_(lambda h: h.reciprocal(out=wsum[:, 0:n_], in_=wsum[:, 0:n_]))
            V(lambda h: h.tensor_tensor(out=gates[:, 0:n_, :], in0=wgt[:, 0:n_, :], in1=wsum[:, 0:n_, None].to_broadcast([128, n_, NE]), op=ALU.mult),
              [d_r], [d_gt])
            if debug and l == 0:
                dump("gates", gates, [d_gt])

            wg = [RL.alloc([128, 8, DEXP], BF16) for _ in range(2)]
            wu = [RL.alloc([128, 8, DEXP], BF16) for _ in range(2)]
            wds = [RL.alloc([128, 2, D], F32) for _ in range(2)]
            wdx = [RL.alloc([128, 2, D], BF16) for _ in range(2)]
            RR1x.reset()
            wdc = [RR1x.alloc([128, 2, D], BF16) for _ in range(2)]
            hid = [RR1x.alloc([128, 2, 512], BF16) for _ in range(2)]
            sg = [RR1x.alloc([128, 512], F32) for _ in range(2)]
            d_wg = [Dep("wg0"), Dep("wg1")]
            d_wu = [Dep("wu0"), Dep("wu1")]
            d_wds = [Dep("wds0"), Dep("wds1")]
            d_wdx = [Dep("wdx0"), Dep("wdx1")]
            d_wdc = [Dep("wdc0"), Dep("wdc1")]
            d_hid = [Dep("hid0"), Dep("hid1")]
            d_sg = [Dep("sg0"), Dep("sg1")]
            chunks = [(n * 512, 512) for n in range(4)] + ([] if last else [(SEQ, 256)])

            def load_expert(e):
                sl = e % 2
                S.dma("pool", lambda h: h.dma_start(out=wg[sl], in_=w_gate_d[l, e].rearrange("(kc p) n -> p kc n", p=128)), writes=[d_wg[sl]])
                S.dma("pool", lambda h: h.dma_start(out=wu[sl], in_=w_up_d[l, e].rearrange("(kc p) n -> p kc n", p=128)), writes=[d_wu[sl]])
                S.dma("sp", lambda h: h.dma_start(out=wds[sl], in_=w_down_d[l, e].rearrange("(kc p) n -> p kc n", p=128)), writes=[d_wds[sl]])
                G(lambda h: h.tensor_tensor(out=wdx[sl], in0=wds[sl], in1=gx2[:, None, :].to_broadcast([128, 2, D]), op=ALU.mult),
                  [d_wds[sl], d_gates[2]], [d_wdx[sl]])
                if not last:
                    G(lambda h: h.tensor_tensor(out=wdc[sl], in0=wds[sl], in1=gc2[:, None, :].to_broadcast([128, 2, D]), op=ALU.mult),
                      [d_wds[sl], d_gates[3]], [d_wdc[sl]])

            load_expert(0)

            def emit_gu(e, c0, cn, hb, hc):
                sl = e % 2
                bg = hc * 2
                bu = hc * 2 + 1
                for kc in range(8):
                    PE(lambda h, kc=kc: h.matmul(PS[:, bg, 0:cn], lhsT=wg[sl][:, kc, hc * 128:(hc + 1) * 128],
                                                 rhs=h2T[:, kc, c0:c0 + cn], start=(kc == 0), stop=(kc == 7)),
                       [d_wg[sl], d_h2], [], [d_ps[bg]])
                for kc in range(8):
                    PE(lambda h, kc=kc: h.matmul(PS[:, bu, 0:cn], lhsT=wu[sl][:, kc, hc * 128:(hc + 1) * 128],
                                                 rhs=h2T[:, kc, c0:c0 + cn], start=(kc == 0), stop=(kc == 7)),
                       [d_wu[sl], d_h2], [], [d_ps[bu]])
                A(lambda h: h.activation(out=sg[hc][:, 0:cn], in_=PS[:, bg, 0:cn], func=AF.Silu), [], [d_sg[hc]], [d_ps[bg]])
                V(lambda h: h.tensor_tensor(out=hid[hb][:, hc, 0:cn], in0=PS[:, bu, 0:cn], in1=sg[hc][:, 0:cn], op=ALU.mult),
                  [d_sg[hc]], [d_hid[hb]], [d_ps[bu]])

            def emit_down(e, c0, cn, hb, jts):
                sl = e % 2
                for jt in jts:
                    t = c0 // 128 + jt
                    isx = t < NTL
                    wdd = wdx[sl] if isx else wdc[sl]
                    dwd = d_wdx[sl] if isx else d_wdc[sl]
                    ob = 4 + (jt % 2) * 2
                    for half in range(2):
                        for hc in range(2):
                            PE(lambda h, half=half, hc=hc, jt=jt, wdd=wdd, ob=ob: h.matmul(
                                PS[:, ob + half, :], lhsT=hid[hb][:, hc, jt * 128:(jt + 1) * 128], rhs=wdd[:, hc, half * 512:(half + 1) * 512],
                                start=(hc == 0), stop=(hc == 1)),
                               [d_hid[hb], dwd], [], [d_ps[ob + half]])
                    V(lambda h, ob=ob, t=t: h.scalar_tensor_tensor(out=xres[:, t, :], in0=PS[:, ob:ob + 2, :].rearrange("p a b -> p (a b)"),
                                                                   scalar=gates[:, t, e:e + 1], in1=xres[:, t, :], op0=ALU.mult, op1=ALU.add),
                      [d_gt, d_xres[t]], [d_xres[t]], [d_ps[ob], d_ps[ob + 1]])

            cnt = 0
            prev = None
            for e in range(NE):
                for ci_, (c0, cn) in enumerate(chunks):
                    if ci_ == 1 and e + 1 < NE:
                        load_expert(e + 1)
                    hb = cnt % 2
                    cnt += 1
                    emit_gu(e, c0, cn, hb, 0)
                    if prev is not None:
                        pn = prev[2] // 128
                        emit_down(prev[0], prev[1], prev[2], prev[3], list(range(0, (pn + 1) // 2)))
                    emit_gu(e, c0, cn, hb, 1)
                    if prev is not None:
                        pn = prev[2] // 128
                        emit_down(prev[0], prev[1], prev[2], prev[3], list(range((pn + 1) // 2, pn)))
                    prev = (e, c0, cn, hb)
            emit_down(prev[0], prev[1], prev[2], prev[3], list(range(prev[2] // 128)))
            S.barrier()
            if debug and l == 0:
                dump("xres_moe", xres, d_xres)
            return False

        for l in range(nlayers):
            if layer(l):
                break
        xres = GS.get('xres')

        if stop_after is None:
            RL.reset()
            fing = RL.alloc([128, D], F32)
            S.dma("sp", lambda h: h.dma_start(out=fing, in_=final_g_d.partition_broadcast(128)), writes=[d_fing])
            fj = RL.alloc([128, D], BF16)
            fo = [RL.alloc([128, D], F32) for _ in range(2)]
            fs = RL.alloc([128, NTL, 2], F32)
            d_fj = Dep("fj")
            d_fo = [Dep("fo0"), Dep("fo1")]
            d_fs = [Dep("fs%d" % t) for t in range(NTL)]
            for t in range(NTL):
                A(lambda h, t=t: h.activation(out=fj, in_=xres[:, t, :], func=AF.Square, accum_out=fs[:, t, 0:1]), [d_xres[t]], [d_fj, d_fs[t]])
            A(lambda h: h.activation(out=fs[:, :, 1], in_=fs[:, :, 0], func=AF.Sqrt, scale=1.0 / D, bias=epsD[:, 0:1]), d_fs + [d_const], d_fs)
            V(lambda h: h.reciprocal(out=fs[:, :, 1], in_=fs[:, :, 1]), d_fs, d_fs)
            for t in range(NTL):
                k = t % 2
                V(lambda h, t=t, k=k: h.scalar_tensor_tensor(out=fo[k], in0=xres[:, t, :], scalar=fs[:, t, 1:2], in1=fing, op0=ALU.mult, op1=ALU.mult),
                  [d_xres[t], d_fs[t], d_fing], [d_fo[k]])
                S.dma("sp", lambda h, t=t, k=k: h.dma_start(out=out_d[t * 128:(t + 1) * 128, :], in_=fo[k]), reads=[d_fo[k]], writes=[d_out], own=d_fo[k])
        S.barrier()
        S.run()
    return nc, dbg_outs


def _consts():
    ident = np.eye(128, dtype=np.float32)
    p = np.arange(128)
    d = p % 64
    axis = d // 32
    jf = (d % 32) % 16
    inv_freq = (10000.0 ** (-(np.arange(0, 32, 2, dtype=np.float32)) / 32.0)).astype(np.float32)
    tpos = np.arange(SEQ)
    row = (tpos // 64).astype(np.float32)
    colp = (tpos % 64).astype(np.float32)
    pos = np.where(axis[:, None] == 0, row[None, :], colp[None, :]).astype(np.float32)
    ang = (pos * inv_freq[jf][:, None]).astype(np.float32)
    ropeC = np.cos(ang).astype(np.float32)
    ropeS = np.sin(ang).astype(np.float32)
    kp = np.arange(128)[:, None]
    qp = np.arange(128)[None, :]
    mprev = (qp <= kp).astype(np.float32)
    mnext = (kp <= qp).astype(np.float32)
    blk64 = ((p[:, None] // 64) == (p[None, :] // 64)).astype(np.float32) / 64.0
    return dict(ident=ident, ropeC=ropeC, ropeS=ropeS, mprev=mprev, mnext=mnext, blk64=blk64)


def _col_perm():
    q = list(range(0, 512))
    for base in (512, 768):
        for g in range(2):
            for j in range(2):
                s = base + (j * 2 + g) * 64
                q += list(range(s, s + 64))
    kA = list(range(1024, 1536))
    vA = list(range(1536, 2048))
    kB = list(range(2048, 2176))
    vB = list(range(2176, 2304))
    kC = list(range(2304, 2432))
    vC = list(range(2432, 2560))
    return np.array(q + kA + kB + kC + vA + vB + vC)


def prepare_in_maps(inputs):
    f = lambda a: np.ascontiguousarray(np.asarray(a, dtype=np.float32))
    x = f(inputs["x"]); c = f(inputs["c"]); ctx = f(inputs["ctx"]); c_ctx = f(inputs["c_ctx"])
    consts = _consts()
    perm = _col_perm()
    w_in_p = np.ascontiguousarray(f(inputs["w_in"])[:, :, perm])
    ng = np.stack([f(inputs["norm1_g"]), f(inputs["norm2_g"])], axis=1)
    ng = np.ascontiguousarray(ng.reshape(DEPTH, 2, 8, 128).transpose(3, 0, 1, 2))
    lamv = np.ascontiguousarray(np.stack([f(inputs["lam_q1"]), f(inputs["lam_k1"]), f(inputs["lam_q2"]), f(inputs["lam_k2"])], axis=1)).reshape(DEPTH, 256)
    dd = np.arange(128) % 64
    i32 = dd % 32
    permd = np.where(i32 < 16, dd + 16, dd - 16)
    qn = f(inputs["q_norm_g"]); kn = f(inputs["k_norm_g"])
    qkn = np.stack([qn[:, dd], qn[:, permd], kn[:, dd], kn[:, permd]], axis=-1)
    qkn = np.ascontiguousarray(qkn.transpose(1, 0, 2))
    shared = dict(
        w_ada=f(inputs["w_ada"]), b_ada=f(inputs["b_ada"]), ng=ng, final_g=f(inputs["final_g"]), w_in_p=w_in_p,
        w_out=f(inputs["w_out"]), lamv=lamv, subln_g=f(inputs["subln_g"]), qkn=qkn, sink=f(inputs["sink"]),
        w_router=f(inputs["w_router"]), router_bias=f(inputs["router_bias"]), w_gate=f(inputs["w_gate"]), w_up=f(inputs["w_up"]),
        w_down=f(inputs["w_down"]), **consts)
    in_maps = []
    for b in range(8):
        cc = np.stack([c[b].reshape(8, 128).T, c_ctx.reshape(8, 128).T], axis=-1)
        m = dict(shared)
        m.update(x=np.ascontiguousarray(x[b]), ctx=np.ascontiguousarray(ctx[b]), cc=np.ascontiguousarray(cc))
        in_maps.append(m)
    return in_maps


_CACHE = {}


def kernel(**inputs):
    in_maps = prepare_in_maps(inputs)
    if "nc" not in _CACHE:
        _CACHE["nc"] = build_program()[0]
    nc = _CACHE["nc"]
    res = run_bass_kernel_spmd(nc, in_maps, core_ids=list(range(8)))
    out = np.stack([np.asarray(r["out"], dtype=np.float32) for r in res.results], axis=0)
    return out
```

```python
import contextlib
import math
import numpy as np
import concourse.bass as bass
import concourse.mybir as mybir
from concourse.bass_utils import run_bass_kernel_spmd
from concourse.alu_op_type import AluOpType as ALU

AF = mybir.ActivationFunctionType
F32 = mybir.dt.float32
BF16 = mybir.dt.bfloat16
AX = mybir.AxisListType

D = 1024
SEQ = 2048
CTXL = 256
T = SEQ + CTXL
NT = 18
NTL = 16
DEPTH = 2
NE = 16
DEXP = 256
RMS_EPS = 1e-6
SBUF_BYTES = 212000


class Dep:
    __slots__ = ("name", "w", "r", "sem", "cnt")

    def __init__(self, name):
        self.name = name
        self.w = None
        self.r = {}
        self.sem = None
        self.cnt = 0


class Sched:
    ENGS = ("pe", "act", "dve", "pool", "sp")

    def __init__(self, nc, stack):
        self.nc = nc
        self.stack = stack
        self.streams = {e: [] for e in self.ENGS}
        self.count = {e: 0 for e in self.ENGS}
        self.seen = {e: {} for e in self.ENGS}
        self.semh = {}
        self.nsem = 0
        self.dma_deps = []
        for e in self.ENGS:
            self.semh[e] = stack.enter_context(nc.semaphore("s_" + e))
            self.nsem += 1
        self.ninstr = 0

    def new_sem(self, name):
        key = "d%d_%s" % (self.nsem, name)
        self.semh[key] = self.stack.enter_context(self.nc.semaphore(key))
        self.nsem += 1
        return key

    def _waits(self, eng, reads, writes, banks=()):
        need = {}
        for d in banks:
            if d.w is not None and d.w[0] != eng:
                s, v = d.w
                if need.get(s, 0) < v:
                    need[s] = v
        for d in reads:
            if d.w is not None:
                s, v = d.w
                if need.get(s, 0) < v:
                    need[s] = v
        for d in writes:
            if d.w is not None:
                s, v = d.w
                if need.get(s, 0) < v:
                    need[s] = v
            for s, v in d.r.items():
                if need.get(s, 0) < v:
                    need[s] = v
        out = []
        seen = self.seen[eng]
        for s, v in need.items():
            if s == eng and eng == "pe":
                continue
            if seen.get(s, 0) >= v:
                continue
            seen[s] = v
            out.append((s, v))
        return out

    def op(self, eng, fn, reads=(), writes=(), banks=()):
        waits = self._waits(eng, reads, writes, banks)
        self.count[eng] += 1
        val = self.count[eng]
        semh = self.semh
        esem = semh[eng]

        def emit(h, waits=waits, fn=fn):
            for s, v in waits:
                h.wait_ge(semh[s], v)
            fn(h).then_inc(esem, 1)

        self.streams[eng].append(emit)
        for d in reads:
            if d.r.get(eng, 0) < val:
                d.r[eng] = val
        for d in writes:
            d.w = (eng, val)
            d.r = {}
        for d in banks:
            d.w = (eng, val)
        self.ninstr += 1

    def dma(self, q, fn, reads=(), writes=(), own=None):
        if own is None:
            own = writes[0]
        if own.sem is None:
            own.sem = self.new_sem(own.name)
            self.dma_deps.append(own)
        waits = self._waits(q, reads, writes)
        own.cnt += 16
        val = own.cnt
        semh = self.semh
        osem = semh[own.sem]

        def emit(h, waits=waits, fn=fn):
            for s, v in waits:
                h.wait_ge(semh[s], v)
            fn(h).then_inc(osem, 16)

        self.streams[q].append(emit)
        for d in reads:
            if d.r.get(own.sem, 0) < val:
                d.r[own.sem] = val
        for d in writes:
            d.w = (own.sem, val)
            d.r = {}
        self.ninstr += 1

    def barrier(self):
        semh = self.semh
        waits = []
        seen = self.seen["sp"]
        for e in self.ENGS:
            if e != "sp" and seen.get(e, 0) < self.count[e]:
                waits.append((e, self.count[e]))
        for d in self.dma_deps:
            if seen.get(d.sem, 0) < d.cnt:
                waits.append((d.sem, d.cnt))
        self.count["sp"] += 1
        val = self.count["sp"]
        spsem = semh["sp"]

        def emit(h, waits=waits):
            for s, v in waits:
                h.wait_ge(semh[s], v)
            h.nop().then_inc(spsem, 1)

        self.streams["sp"].append(emit)
        snap = {e: self.count[e] for e in self.ENGS}
        for d in self.dma_deps:
            snap[d.sem] = d.cnt
        for e in self.ENGS:
            if e != "sp":
                self.streams[e].append(lambda h, val=val: h.wait_ge(spsem, val))
            self.seen[e] = dict(snap)

    def final_wait(self, eng, deps):
        waits = self._waits(eng, deps, ())
        semh = self.semh

        def emit(h, waits=waits):
            for s, v in waits:
                h.wait_ge(semh[s], v)

        self.streams[eng].append(emit)

    def run(self):
        nc = self.nc
        streams = self.streams
        with nc.Block() as block:
            @block.tensor
            def _(h):
                for f in streams["pe"]:
                    f(h)

            @block.scalar
            def _(h):
                for f in streams["act"]:
                    f(h)

            @block.vector
            def _(h):
                for f in streams["dve"]:
                    f(h)

            @block.gpsimd
            def _(h):
                for f in streams["pool"]:
                    f(h)

            @block.sync
            def _(h):
                for f in streams["sp"]:
                    f(h)


class Region:
    def __init__(self, sb, base, size):
        self.sb = sb
        self.base = base
        self.size = size
        self.off = 0

    def reset(self):
        self.off = 0

    def alloc(self, shape, dt):
        esz = 4 if dt == F32 else 2
        n = 1
        for s in shape[1:]:
            n *= s
        nbytes = (n * esz + 31) // 32 * 32
        assert self.off + nbytes <= self.size, ("region overflow", self.off, nbytes, self.size)
        b = self.base + self.off
        self.off += nbytes
        ap = self.sb[:, b // 2:(b + n * esz) // 2]
        if dt != BF16:
            ap = ap.bitcast(dt)
        if len(shape) == 3:
            ap = ap.rearrange("p (a b) -> p a b", a=shape[1])
        elif len(shape) == 4:
            ap = ap.rearrange("p (a b c) -> p a b c", a=shape[1], b=shape[2])
        if shape[0] != 128:
            ap = ap[0:shape[0]]
        return ap


def build_program(debug=False, nlayers=DEPTH, stop_after=None):
    nc = bass.Bass("TRN2", target_bir_lowering=False)
    din = lambda name, shape: nc.dram_tensor(name, shape, F32, kind="ExternalInput").ap()
    x_d = din("x", [SEQ, D])
    ctx_d = din("ctx", [CTXL, D])
    cc_d = din("cc", [128, 8, 2])
    w_ada_d = din("w_ada", [DEPTH, D, 6 * D])
    b_ada_d = din("b_ada", [DEPTH, 6 * D])
    ng_d = din("ng", [128, DEPTH, 2, 8])
    final_g_d = din("final_g", [D])
    w_in_d = din("w_in_p", [DEPTH, D, 2560])
    w_out_d = din("w_out", [DEPTH, D, D])
    lamv_d = din("lamv", [DEPTH, 256])
    subln_d = din("subln_g", [DEPTH, 128])
    qkn_d = din("qkn", [128, DEPTH, 4])
    sink_d = din("sink", [DEPTH, 4])
    w_router_d = din("w_router", [D, NE])
    rbias_d = din("router_bias", [NE])
    w_gate_d = din("w_gate", [DEPTH, NE, D, DEXP])
    w_up_d = din("w_up", [DEPTH, NE, D, DEXP])
    w_down_d = din("w_down", [DEPTH, NE, DEXP, D])
    ident_d = din("ident", [128, 128])
    ropeC_d = din("ropeC", [128, SEQ])
    ropeS_d = din("ropeS", [128, SEQ])
    mprev_d = din("mprev", [128, 128])
    mnext_d = din("mnext", [128, 128])
    blk64_d = din("blk64", [128, 128])
    out_d = nc.dram_tensor("out", [SEQ, D], F32, kind="ExternalOutput").ap()
    xsp_d = nc.dram_tensor("xsp", [SEQ, D], F32, kind="Internal").ap()
    dbg_outs = {}

    with contextlib.ExitStack() as st:
        S = Sched(nc, st)
        SB = st.enter_context(nc.sbuf_tensor("SB", [128, SBUF_BYTES // 2], BF16))
        PS = st.enter_context(nc.psum_tensor("PS", [128, 8, 512], F32))
        P_SIZE = 24576
        R2_SIZE = 36864
        R1_SIZE = 92736
        L_SIZE = SBUF_BYTES - P_SIZE - R2_SIZE - R1_SIZE
        RP = Region(SB, 0, P_SIZE)
        RR2 = Region(SB, P_SIZE, R2_SIZE)
        RR1 = Region(SB, P_SIZE + R2_SIZE, R1_SIZE)
        RL = Region(SB, P_SIZE + R2_SIZE + R1_SIZE, L_SIZE)

        def V(fn, r=(), w=(), b=()):
            S.op("dve", fn, r, w, b)

        def A(fn, r=(), w=(), b=()):
            S.op("act", fn, r, w, b)

        def PE(fn, r=(), w=(), b=()):
            S.op("pe", fn, r, w, b)

        def G(fn, r=(), w=()):
            S.op("pool", fn, r, w)

        def dump(name, ap, deps, dt=F32):
            if not debug:
                return
            shape = list(ap.shape)
            t = nc.dram_tensor("dbg_" + name, shape, dt, kind="ExternalOutput").ap()
            dd = Dep("dbg_" + name)
            dbg_outs[name] = dd
            S.dma("sp", lambda h: h.dma_start(out=t, in_=ap), reads=deps, writes=[dd])

        identf = RP.alloc([128, 128], F32)
        identb = RP.alloc([128, 128], BF16)
        mprevb = RP.alloc([128, 128], BF16)
        mnextb = RP.alloc([128, 128], BF16)
        mprevn = RP.alloc([128, 128], BF16)
        mnextn = RP.alloc([128, 128], BF16)
        blk64 = RP.alloc([128, 128], F32)
        mhalf = RP.alloc([128, 512], F32)
        epsD = RP.alloc([128, 1], F32)
        ccf = RP.alloc([128, 8, 2], F32)
        scT = RP.alloc([128, 8, 2], BF16)
        ng = RP.alloc([128, DEPTH, 2, 8], F32)
        bfm = RP.alloc([128, 48], F32)
        modfm = RP.alloc([128, 48, 2], F32)
        scale1 = RP.alloc([128, 8, 2], F32)
        scale2 = RP.alloc([128, 8, 2], F32)
        gx1 = RP.alloc([128, D], F32)
        gc1 = RP.alloc([128, D], F32)
        gx2 = RP.alloc([128, D], F32)
        gc2 = RP.alloc([128, D], F32)
        wrt = RP.alloc([128, 8, NE], BF16)
        rbias = RP.alloc([128, NE], F32)
        lamt = RP.alloc([128, 4, 64], F32)
        lamt_flat = lamt.rearrange('p a b -> p (a b)')
        lamj = RP.alloc([128, 64], F32)
        lams = RP.alloc([128, 2], F32)
        neglam = RP.alloc([128, 1], F32)
        gA = RP.alloc([128, 128], F32)
        qkn = RP.alloc([128, DEPTH, 4], F32)
        sinkt = RP.alloc([128, 4], F32)
        esink = RP.alloc([128, 4], F32)

        d_const = Dep("const")
        d_cc = Dep("cc")
        d_scT = Dep("scT")
        d_ng = Dep("ng")
        d_mod = Dep("mod")
        d_gates = [Dep("g%d" % i) for i in range(4)]
        d_wrt = Dep("wrt")
        d_rb = Dep("rb")
        d_lam = Dep("lam")
        d_gA = Dep("gA")
        d_qkn = Dep("qkn")
        d_sink = Dep("sink")
        d_fing = Dep("fing")
        d_hT = Dep("hT")
        d_xres = [Dep("xres%d" % t) for t in range(NT)]
        d_ps = [Dep("ps%d" % i) for i in range(8)]
        d_xsp = Dep("xsp")
        d_out = Dep("out")

        d_idf = Dep("idf")
        d_mpf = Dep("mpf")
        tmpc = RL.alloc([128, 3, 128], F32)
        S.dma("sp", lambda h: h.dma_start(out=identf, in_=ident_d), writes=[d_idf])
        S.dma("sp", lambda h: h.dma_start(out=tmpc[:, 0, :], in_=mprev_d), writes=[d_mpf])
        S.dma("sp", lambda h: h.dma_start(out=tmpc[:, 1, :], in_=mnext_d), writes=[d_mpf])
        S.dma("sp", lambda h: h.dma_start(out=blk64, in_=blk64_d), writes=[d_const])
        S.dma("sp", lambda h: h.dma_start(out=ccf, in_=cc_d), writes=[d_cc])
        S.dma("sp", lambda h: h.dma_start(out=ng, in_=ng_d), writes=[d_ng])
        S.dma("sp", lambda h: h.dma_start(out=qkn, in_=qkn_d), writes=[d_qkn])
        S.dma("sp", lambda h: h.dma_start(out=rbias, in_=rbias_d.partition_broadcast(128)), writes=[d_rb])
        S.dma("pool", lambda h: h.dma_start(out=wrt, in_=w_router_d.rearrange("(kc p) n -> p kc n", p=128)), writes=[d_wrt])
        V(lambda h: h.tensor_copy(out=identb, in_=identf), [d_idf], [d_const])
        V(lambda h: h.tensor_copy(out=mprevb, in_=tmpc[:, 0, :]), [d_mpf], [d_const])
        V(lambda h: h.tensor_copy(out=mnextb, in_=tmpc[:, 1, :]), [d_mpf], [d_const])
        V(lambda h: h.tensor_scalar(out=mprevn, in0=tmpc[:, 0, :], scalar1=-1.0, scalar2=30000.0, op0=ALU.add, op1=ALU.mult), [d_mpf], [d_const])
        V(lambda h: h.tensor_scalar(out=mnextn, in0=tmpc[:, 1, :], scalar1=-1.0, scalar2=30000.0, op0=ALU.add, op1=ALU.mult), [d_mpf], [d_const])
        V(lambda h: h.memset(mhalf, -0.5), [], [d_const])
        V(lambda h: h.memset(epsD, RMS_EPS), [], [d_const])
        A(lambda h: h.activation(out=ccf, in_=ccf, func=AF.Silu), [d_cc], [d_cc])
        V(lambda h: h.tensor_copy(out=scT, in_=ccf), [d_cc], [d_scT])
        S.barrier()

        def rms_to_featmajor(src_tile_fn, tiles_groups, scale_t, bias_lo, dst, d_dst, src_dep_fn, loader=None):
            RL.reset()
            junk = RL.alloc([128, D], BF16)
            yn = [RL.alloc([128, D], BF16) for _ in range(8)]
            ssq = RL.alloc([128, NT], F32)
            rst = RL.alloc([128, NT], F32)
            xs = [RL.alloc([128, D], F32) for _ in range(6)]
            d_xs = [Dep("xs%d" % i) for i in range(6)]
            nload = [0]
            d_junk = Dep("junk")
            d_yn = [Dep("yn%d" % i) for i in range(8)]
            d_ssq = [Dep("ssq%d" % i) for i in range(NT)]
            d_rst = [Dep("rst%d" % i) for i in range(NT)]
            def stage_a(gi):
                tiles = tiles_groups[gi]
                par = gi % 2
                srcs = []
                for i, t in enumerate(tiles):
                    if loader is not None:
                        k3 = nload[0] % 6
                        nload[0] += 1
                        loader(t, xs[k3], d_xs[k3])
                        src = xs[k3]
                        sd = d_xs[k3]
                    else:
                        src = src_tile_fn(t)
                        sd = src_dep_fn(t)
                    A(lambda h, src=src, t=t: h.activation(out=junk, in_=src, func=AF.Square, accum_out=ssq[:, t:t + 1]),
                      [sd], [d_junk, d_ssq[t]])
                    srcs.append((src, sd))
                ta_, tb2_ = tiles[0], tiles[-1] + 1
                A(lambda h: h.activation(out=rst[:, ta_:tb2_], in_=ssq[:, ta_:tb2_], func=AF.Sqrt, scale=1.0 / D, bias=epsD[:, 0:1]),
                  [d_ssq[t] for t in tiles] + [d_const], [d_rst[t] for t in tiles])
                V(lambda h: h.reciprocal(out=rst[:, ta_:tb2_], in_=rst[:, ta_:tb2_]), [d_rst[t] for t in tiles], [d_rst[t] for t in tiles])
                for i, t in enumerate(tiles):
                    src, sd = srcs[i]
                    yi = par * 4 + i
                    V(lambda h, src=src, t=t, yi=yi: h.tensor_scalar(out=yn[yi], in0=src, scalar1=rst[:, t:t + 1], scalar2=None, op0=ALU.mult),
                      [sd, d_rst[t]], [d_yn[yi]])

            def stage_b(gi):
                tiles = tiles_groups[gi]
                par = gi % 2
                n = len(tiles)
                col = 0 if tiles[0] < NTL else 1
                t0 = tiles[0]
                for c in range(8):
                    psb = PS[:, c, :].bitcast(BF16)
                    for i, t in enumerate(tiles):
                        yi = par * 4 + i
                        PE(lambda h, psb=psb, yi=yi, c=c, i=i: h.transpose(
                            out=psb[:, i * 128:(i + 1) * 128], in_=yn[yi][:, c * 128:(c + 1) * 128], identity=identb),
                           [d_yn[yi], d_const], [], [d_ps[c]])
                    src = psb[:, 0:n * 128]
                    dsl = dst[:, c, t0 * 128:(t0 + n) * 128]
                    if c % 2 == 0:
                        A(lambda h, src=src, dsl=dsl, c=c: h.activation(out=dsl, in_=src, func=AF.Identity,
                                                                        scale=scale_t[:, c, col:col + 1],
                                                                        bias=modfm[:, bias_lo + c, col:col + 1]),
                          [d_mod], [d_dst], [d_ps[c]])
                    else:
                        V(lambda h, src=src, dsl=dsl, c=c: h.tensor_scalar(out=dsl, in0=src, scalar1=scale_t[:, c, col:col + 1],
                                                                           scalar2=modfm[:, bias_lo + c, col:col + 1],
                                                                           op0=ALU.mult, op1=ALU.add),
                          [d_mod], [d_dst], [d_ps[c]])

            ng_ = len(tiles_groups)
            stage_a(0)
            for gi in range(ng_):
                if gi + 1 < ng_:
                    stage_a(gi + 1)
                stage_b(gi)

        hT = RR2.alloc([128, 8, T], BF16)

        GS = {}

        def layer(l):
            xres = GS.get('xres')
            last = (l == DEPTH - 1)
            lambda_init = 0.8 - 0.6 * math.exp(-0.3 * l)
            tok_tiles = list(range(NT)) if not last else list(range(NTL))
            RL.reset()
            wa = [RL.alloc([128, 8, 512], BF16) for _ in range(3)]
            scB = RL.alloc([128, 2, 8, 128], BF16)
            d_wa = [Dep("wa%d" % i) for i in range(3)]
            d_scB = Dep("scB")
            d_bfm = Dep("bfm")
            d_psfm = Dep("psfm")
            d_psbc = [Dep("psbc%d" % i) for i in range(4)]
            for i in range(6):
                S.dma("sp", lambda h, i=i: h.dma_start(out=bfm[:, i * 8:(i + 1) * 8],
                                                       in_=b_ada_d[l, i * 1024:(i + 1) * 1024].rearrange("(c p) -> p c", p=128),
                                                       allow_slow_non_contiguous=True), writes=[d_bfm])
            for gi, (gt, lo) in enumerate(((gx1, 2048), (gc1, 2048), (gx2, 5120), (gc2, 5120))):
                S.dma("sp", lambda h, gt=gt, lo=lo: h.dma_start(out=gt, in_=b_ada_d[l, lo:lo + 1024].partition_broadcast(128)),
                      writes=[d_gates[gi]])
            for j in range(2):
                V(lambda h, j=j: h.tensor_copy(out=scB[:, j, :, :], in_=scT[:, :, j:j + 1].to_broadcast([128, 8, 128])), [d_scT], [d_scB])
            psfm = PS[:, 0, 0:96]
            for j in range(12):
                wj = wa[j % 3]
                dwj = d_wa[j % 3]
                S.dma("pool", lambda h, wj=wj, j=j: h.dma_start(out=wj, in_=w_ada_d[l, :, j * 512:(j + 1) * 512].rearrange("(kc p) n -> p kc n", p=128)),
                      writes=[dwj])
                for f in range(4):
                    fc = j * 4 + f
                    for kc in range(8):
                        PE(lambda h, wj=wj, f=f, kc=kc, fc=fc: h.matmul(psfm[:, fc * 2:fc * 2 + 2], lhsT=wj[:, kc, f * 128:(f + 1) * 128],
                                                                        rhs=scT[:, kc, :], start=(kc == 0), stop=(kc == 7)),
                           [dwj, d_scT], [], [d_ps[0]])
                if j in (4, 5, 10, 11):
                    half = j % 2 if j < 6 else (j - 10)
                    for who in range(2):
                        if who == 1 and last:
                            continue
                        gi = (0 if j < 6 else 2) + who
                        gt = (gx1, gc1, gx2, gc2)[gi]
                        bank = 1 + (half * 2 + who)
                        for kc in range(8):
                            PE(lambda h, wj=wj, kc=kc, who=who, bank=bank: h.matmul(PS[:, bank, :], lhsT=scB[:, who, kc, :], rhs=wj[:, kc, :],
                                                                                    start=(kc == 0), stop=(kc == 7)),
                               [dwj, d_scB], [], [d_ps[bank]])
                        V(lambda h, gt=gt, bank=bank, half=half: h.tensor_tensor(out=gt[:, half * 512:(half + 1) * 512], in0=PS[:, bank, :],
                                                                                 in1=gt[:, half * 512:(half + 1) * 512], op=ALU.add),
                          [d_gates[gi]], [d_gates[gi]], [d_ps[bank]])
            V(lambda h: h.tensor_tensor(out=modfm, in0=psfm.rearrange("p (c j) -> p c j", j=2), in1=bfm[:, :, None].to_broadcast([128, 48, 2]),
                                        op=ALU.add), [d_bfm], [d_mod], [d_ps[0]])
            V(lambda h: h.scalar_tensor_tensor(out=scale1, in0=modfm[:, 8:16, :], scalar=1.0,
                                               in1=ng[:, l, 0, :, None].to_broadcast([128, 8, 2]), op0=ALU.add, op1=ALU.mult),
              [d_mod, d_ng], [d_mod])
            V(lambda h: h.scalar_tensor_tensor(out=scale2, in0=modfm[:, 32:40, :], scalar=1.0,
                                               in1=ng[:, l, 1, :, None].to_broadcast([128, 8, 2]), op0=ALU.add, op1=ALU.mult),
              [d_mod, d_ng], [d_mod])
            S.dma("sp", lambda h: h.dma_start(out=lamt_flat, in_=lamv_d[l].partition_broadcast(128)), writes=[d_lam])
            S.dma("sp", lambda h: h.dma_start(out=gA, in_=subln_d[l].partition_broadcast(128)), writes=[d_gA])
            S.dma("sp", lambda h: h.dma_start(out=sinkt, in_=sink_d[l].partition_broadcast(128)), writes=[d_sink])
            for i in range(2):
                V(lambda h, i=i: h.tensor_tensor(out=lamj, in0=lamt[:, 2 * i, :], in1=lamt[:, 2 * i + 1, :], op=ALU.mult), [d_lam], [d_lam])
                V(lambda h, i=i: h.tensor_reduce(out=lams[:, i:i + 1], in_=lamj, axis=AX.X, op=ALU.add), [d_lam], [d_lam])
            A(lambda h: h.activation(out=lams, in_=lams, func=AF.Exp), [d_lam], [d_lam])
            V(lambda h: h.scalar_tensor_tensor(out=neglam, in0=lams[:, 1:2], scalar=-lambda_init, in1=lams[:, 0:1], op0=ALU.add, op1=ALU.subtract),
              [d_lam], [d_lam])
            V(lambda h: h.tensor_scalar(out=gA, in0=gA, scalar1=(1.0 - lambda_init), scalar2=None, op0=ALU.mult), [d_gA], [d_gA])
            A(lambda h: h.activation(out=esink, in_=sinkt, func=AF.Exp), [d_sink], [d_sink])
            S.barrier()
            if debug and l == 0:
                dump("modfm", modfm, [d_mod])
                dump("gx1", gx1, [d_gates[0]])
                dump("gc2", gc2, [d_gates[3]])

            groups = [[0, 1, 2, 3], [4, 5, 6, 7], [8, 9, 10, 11], [12, 13, 14, 15], [16, 17]]
            if l == 0:
                def loader(t, dst_ap, dst_dep):
                    srcd = x_d[t * 128:(t + 1) * 128, :] if t < NTL else ctx_d[(t - NTL) * 128:(t - NTL + 1) * 128, :]
                    S.dma("sp", lambda h, srcd=srcd, dst_ap=dst_ap: h.dma_start(out=dst_ap, in_=srcd), writes=[dst_dep])

                rms_to_featmajor(None, groups, scale1, 0, hT, d_hT, None, loader=loader)
            else:
                rms_to_featmajor(lambda t: xres[:, t, :], groups, scale1, 0, hT, d_hT, lambda t: d_xres[t])
                for q4 in range(4):
                    S.dma("sp", lambda h, q4=q4: h.dma_start(out=xsp_d[q4 * 512:(q4 + 1) * 512, :].rearrange("(t p) d -> p t d", p=128),
                                                             in_=xres[:, q4 * 4:(q4 + 1) * 4, :]),
                          reads=d_xres[q4 * 4:(q4 + 1) * 4], writes=[d_xsp])
            S.barrier()
            if debug and l == 0:
                dump("hT", hT, [d_hT], BF16)
            if stop_after == "p1":
                return True

            RR1.reset()
            qk = RR1.alloc([128, 14, T], BF16)
            vt = RR1.alloc([128, NT, 784], BF16)
            RL.reset()
            ropeC = RL.alloc([128, SEQ], BF16)
            ropeS = RL.alloc([128, SEQ], BF16)
            wq = [RL.alloc([128, 8, 128], BF16) for _ in range(3)]
            wr = [RL.alloc([128, 8, 128], BF16) for _ in range(3)]
            wv = RL.alloc([128, 8, 768], BF16)
            t1 = [RL.alloc([128, 512], F32) for _ in range(2)]
            t2 = [RL.alloc([128, 512], F32) for _ in range(2)]
            sqb = [RL.alloc([128, 512], F32) for _ in range(2)]
            rsb = [RL.alloc([128, 512], F32) for _ in range(2)]
            d_rope = Dep("rope")
            d_wq = [Dep("wq%d" % i) for i in range(3)]
            d_wr = [Dep("wr%d" % i) for i in range(3)]
            d_wv = Dep("wv")
            d_t1 = [Dep("t1%d" % i) for i in range(2)]
            d_t2 = [Dep("t2%d" % i) for i in range(2)]
            d_sq = [Dep("sq%d" % i) for i in range(2)]
            d_rs = [Dep("rs%d" % i) for i in range(2)]
            d_qk = [Dep("qk%d" % i) for i in range(14)]
            d_v = [Dep("v%d" % i) for i in range(NT)]
            d_vones = Dep("vones")
            S.dma("pool", lambda h: h.dma_start(out=ropeC, in_=ropeC_d), writes=[d_rope])
            S.dma("pool", lambda h: h.dma_start(out=ropeS, in_=ropeS_d), writes=[d_rope])
            S.dma("pool", lambda h: h.dma_start(out=wv, in_=w_in_d[l, :, 1792:2560].rearrange("(kc p) n -> p kc n", p=128)), writes=[d_wv])
            for t in range(NT):
                G(lambda h, t=t: h.memset(vt[:, t, :], 1.0), [], [d_v[t]])
            it = 0
            wst = [RL.alloc([128, 8, 128], F32) for _ in range(2)]
            d_wst = [Dep("wst0"), Dep("wst1")]

            def load_w(ci):
                sl = ci % 3
                ss = ci % 2
                S.dma("sp", lambda h: h.dma_start(out=wst[ss], in_=w_in_d[l, :, ci * 128:(ci + 1) * 128].rearrange("(kc p) n -> p kc n", p=128)),
                      writes=[d_wst[ss]])
                ws5 = wst[ss].rearrange("p k (u two s) -> p k u two s", two=2, s=16)
                wr5 = wr[sl].rearrange("p k (u two s) -> p k u two s", two=2, s=16)
                A(lambda h: h.activation(out=wq[sl], in_=wst[ss], func=AF.Copy), [d_wst[ss]], [d_wq[sl]])
                for kh in range(2):
                    A(lambda h, kh=kh: h.activation(out=wr5[:, kh * 4:(kh + 1) * 4, :, 0, :], in_=ws5[:, kh * 4:(kh + 1) * 4, :, 1, :], func=AF.Copy, scale=-1.0),
                      [d_wst[ss]], [d_wr[sl]])
                    A(lambda h, kh=kh: h.activation(out=wr5[:, kh * 4:(kh + 1) * 4, :, 1, :], in_=ws5[:, kh * 4:(kh + 1) * 4, :, 0, :], func=AF.Copy),
                      [d_wst[ss]], [d_wr[sl]])

            load_w(0)
            load_w(1)
            for ci in range(14):
                sl = ci % 3
                if ci + 2 < 14:
                    load_w(ci + 2)
                isB = ci in (4, 5, 12)
                gcol = 0 if ci < 8 else 2
                for n in range(4):
                    pa = (it % 2) * 2
                    pb = pa + 1
                    tb = it % 2
                    it += 1
                    cs = slice(n * 512, (n + 1) * 512)
                    for kc in range(8):
                        PE(lambda h, pa=pa, sl=sl, kc=kc, cs=cs: h.matmul(PS[:, pa, :], lhsT=wq[sl][:, kc, :], rhs=hT[:, kc, cs], start=(kc == 0), stop=(kc == 7)),
                           [d_wq[sl], d_hT], [], [d_ps[pa]])
                    for kc in range(8):
                        PE(lambda h, pb=pb, sl=sl, kc=kc, cs=cs: h.matmul(PS[:, pb, :], lhsT=wr[sl][:, kc, :], rhs=hT[:, kc, cs], start=(kc == 0), stop=(kc == 7)),
                           [d_wr[sl], d_hT], [], [d_ps[pb]])
                    if not isB:
                        V(lambda h, pa=pa, tb=tb, cs=cs: h.tensor_tensor(out=t1[tb], in0=PS[:, pa, :], in1=ropeC[:, cs], op=ALU.mult),
                          [d_rope], [d_t1[tb]], [d_ps[pa]])
                        V(lambda h, pb=pb, tb=tb, cs=cs: h.tensor_tensor(out=t2[tb], in0=PS[:, pb, :], in1=ropeS[:, cs], op=ALU.mult),
                          [d_rope], [d_t2[tb]], [d_ps[pb]])
                        G(lambda h, tb=tb, ci=ci, cs=cs: h.tensor_tensor(out=qk[:, ci, cs], in0=t1[tb], in1=t2[tb], op=ALU.add),
                          [d_t1[tb], d_t2[tb]], [d_qk[ci]])
                    else:
                        A(lambda h, pa=pa, tb=tb: h.activation(out=sqb[tb], in_=PS[:, pa, :], func=AF.Square), [], [d_sq[tb]], [d_ps[pa]])
                        pm = 4 + tb
                        PE(lambda h, pm=pm, tb=tb: h.matmul(PS[:, pm, :], lhsT=blk64, rhs=sqb[tb], start=True, stop=True),
                           [d_sq[tb], d_const], [], [d_ps[pm]])
                        A(lambda h, pm=pm, tb=tb: h.activation(out=rsb[tb], in_=PS[:, pm, :], func=AF.Sqrt, bias=epsD[:, 0:1]),
                          [d_const], [d_rs[tb]], [d_ps[pm]])
                        V(lambda h, tb=tb: h.reciprocal(out=rsb[tb], in_=rsb[tb]), [d_rs[tb]], [d_rs[tb]])
                        V(lambda h, pa=pa, tb=tb, cs=cs, gcol=gcol: h.scalar_tensor_tensor(out=t1[tb], in0=PS[:, pa, :], scalar=qkn[:, l, gcol:gcol + 1],
                                                                                          in1=ropeC[:, cs], op0=ALU.mult, op1=ALU.mult),
                          [d_rope, d_qkn], [d_t1[tb]], [d_ps[pa]])
                        V(lambda h, pb=pb, tb=tb, cs=cs, gcol=gcol: h.scalar_tensor_tensor(out=t2[tb], in0=PS[:, pb, :], scalar=qkn[:, l, gcol + 1:gcol + 2],
                                                                                          in1=ropeS[:, cs], op0=ALU.mult, op1=ALU.mult),
                          [d_rope, d_qkn], [d_t2[tb]], [d_ps[pb]])
                        G(lambda h, tb=tb: h.tensor_tensor(out=t1[tb], in0=t1[tb], in1=t2[tb], op=ALU.add), [d_t1[tb], d_t2[tb]], [d_t1[tb]])
                        G(lambda h, tb=tb, ci=ci, cs=cs: h.tensor_tensor(out=qk[:, ci, cs], in0=t1[tb], in1=rsb[tb], op=ALU.mult),
                          [d_t1[tb], d_rs[tb]], [d_qk[ci]])
                if ci >= 8 or not last:
                    pa = (it % 2) * 2
                    tb = it % 2
                    it += 1
                    cs = slice(SEQ, T)
                    for kc in range(8):
                        PE(lambda h, pa=pa, sl=sl, kc=kc, cs=cs: h.matmul(PS[:, pa, 0:CTXL], lhsT=wq[sl][:, kc, :], rhs=hT[:, kc, cs], start=(kc == 0), stop=(kc == 7)),
                           [d_wq[sl], d_hT], [], [d_ps[pa]])
                    if not isB:
                        A(lambda h, pa=pa, ci=ci, cs=cs: h.activation(out=qk[:, ci, cs], in_=PS[:, pa, 0:CTXL], func=AF.Copy), [], [d_qk[ci]], [d_ps[pa]])
                    else:
                        A(lambda h, pa=pa, tb=tb: h.activation(out=sqb[tb][:, 0:CTXL], in_=PS[:, pa, 0:CTXL], func=AF.Square), [], [d_sq[tb]], [d_ps[pa]])
                        pm = 4 + tb
                        PE(lambda h, pm=pm, tb=tb: h.matmul(PS[:, pm, 0:CTXL], lhsT=blk64, rhs=sqb[tb][:, 0:CTXL], start=True, stop=True),
                           [d_sq[tb], d_const], [], [d_ps[pm]])
                        A(lambda h, pm=pm, tb=tb: h.activation(out=rsb[tb][:, 0:CTXL], in_=PS[:, pm, 0:CTXL], func=AF.Sqrt, bias=epsD[:, 0:1]),
                          [d_const], [d_rs[tb]], [d_ps[pm]])
                        V(lambda h, tb=tb: h.reciprocal(out=rsb[tb][:, 0:CTXL], in_=rsb[tb][:, 0:CTXL]), [d_rs[tb]], [d_rs[tb]])
                        V(lambda h, pa=pa, tb=tb, ci=ci, cs=cs, gcol=gcol: h.scalar_tensor_tensor(out=qk[:, ci, cs], in0=PS[:, pa, 0:CTXL],
                                                                                                 scalar=qkn[:, l, gcol:gcol + 1], in1=rsb[tb][:, 0:CTXL],
                                                                                                 op0=ALU.mult, op1=ALU.mult),
                          [d_rs[tb], d_qkn], [d_qk[ci]], [d_ps[pa]])
            for t in range(NT):
                b1 = 6 + (t % 2)
                ts_ = slice(t * 128, (t + 1) * 128)
                for kc in range(8):
                    PE(lambda h, b1=b1, kc=kc, ts_=ts_: h.matmul(PS[:, b1, :], lhsT=hT[:, kc, ts_], rhs=wv[:, kc, 0:512], start=(kc == 0), stop=(kc == 7)),
                       [d_hT, d_wv], [], [d_ps[b1]])
                A(lambda h, b1=b1, t=t: h.activation(out=vt[:, t, 0:520].rearrange("p (h c) -> p h c", c=130)[:, :, 0:128],
                                                     in_=PS[:, b1, :].rearrange("p (h c) -> p h c", c=128), func=AF.Copy),
                  [], [d_v[t]], [d_ps[b1]])
                b2 = 4 + (t % 2)
                for kc in range(8):
                    PE(lambda h, b2=b2, kc=kc, ts_=ts_: h.matmul(PS[:, b2, 0:256], lhsT=hT[:, kc, ts_], rhs=wv[:, kc, 512:768], start=(kc == 0), stop=(kc == 7)),
                       [d_hT, d_wv], [], [d_ps[b2]])
                V(lambda h, b2=b2, t=t: h.tensor_copy(out=vt[:, t, 520:784].rearrange("p (h c) -> p h c", c=66)[:, :, 0:64],
                                                      in_=PS[:, b2, 0:256].rearrange("p (h c) -> p h c", c=64)),
                  [], [d_v[t]], [d_ps[b2]])
            S.barrier()
            if debug and l == 0:
                dump("qk", qk, d_qk, BF16)
                dump("vt", vt, d_v, BF16)
            if stop_after == "p2":
                return True

            mixT = hT
            d_mix = Dep("mixT")
            RL.reset()
            wo = RL.alloc([128, 8, D], BF16)
            d_wo = Dep("wo")
            S.dma("pool", lambda h: h.dma_start(out=wo, in_=w_out_d[l].rearrange("(kc p) n -> p kc n", p=128)), writes=[d_wo])
            Pb = [RL.alloc([128, 2, 512], BF16) for _ in range(3)]
            d_P = [Dep("P%d" % i) for i in range(3)]
            accs_sb = [RL.alloc([128, 3, 512], F32) for _ in range(3)]
            d_accs = [[Dep("accs%d_%d" % (u, k)) for k in range(3)] for u in range(3)]
            eo = [RL.alloc([128, 4, 128], F32) for _ in range(3)]
            et = [RL.alloc([128, 128], F32) for _ in range(2)]
            eon = [RL.alloc([128, 4, 128], BF16) for _ in range(3)]
            ejunk = RL.alloc([128, 128], BF16)
            esm = [RL.alloc([128, 4, 4], F32) for _ in range(3)]
            d_eo = [Dep("eo%d" % i) for i in range(3)]
            d_et = [Dep("et%d" % i) for i in range(2)]
            d_eon = [Dep("eon%d" % i) for i in range(3)]
            d_ej = Dep("ejunk")
            d_esm = [Dep("esm%d" % i) for i in range(3)]
            qz = [RL.alloc([128, 2, 512], BF16) for _ in range(3)]
            d_qz = [Dep("qz0"), Dep("qz1"), Dep("qz2")]
            state = {"sb": 0, "pb": 0, "ep": 0, "u": 0, "zq": 0}

            units = []

            def attn_unit(kind, streams, nq, q0, ktiles, vcol, dv, masks, mix_chunk, sink_cols=None, valid=None):
                units.append(dict(kind=kind, streams=streams, nq=nq, q0=q0, ktiles=ktiles, vcol=vcol, dv=dv, masks=masks,
                                  mix_chunk=mix_chunk, sink_cols=sink_cols, valid=valid))

            def prep_qz(U, zq):
                qzu = qz[zq]
                nq, q0 = U["nq"], U["q0"]
                G(lambda h: h.memset(qzu[:, :, 0:nq], 0.0), [], [d_qz[zq]])
                for s_, (qc, kc_, base) in enumerate(U["streams"]):
                    G(lambda h, s_=s_, qc=qc, base=base: h.tensor_copy(out=qzu[base:base + 64, s_, 0:nq], in_=qk[base:base + 64, qc, q0:q0 + nq]),
                      [d_qk[qc]], [d_qz[zq]])

            class UnitCtx:
                def __init__(self, U, ui):
                    self.U = U
                    self.ui = ui
                    self.zq = ui % 3
                    self.u = ui % 3
                    nq, dv = U["nq"], U["dv"]
                    self.nj = nq // 128
                    self.stride = 130 if dv == 128 else 66
                    self.perbank = 3 if dv == 128 else 4
                    self.accs = []
                    for s_ in range(2):
                        for j in range(self.nj):
                            a = s_ * self.nj + j
                            self.accs.append((a // self.perbank, (a % self.perbank) * self.stride, a))
                    self.nbanks = self.accs[-1][0] + 1
                    self.started = set()

            def emit_s_exp(C, ki):
                U = C.U
                nq, kt = U["nq"], U["ktiles"][ki]
                qzu = qz[C.zq]
                sb = state["sb"]
                state["sb"] ^= 1
                pbi = state["pb"]
                state["pb"] = (pbi + 1) % 3
                ks = slice(kt * 128, (kt + 1) * 128)
                for s_, (qc, kc_, base) in enumerate(U["streams"]):
                    PE(lambda h, s_=s_, kc_=kc_: h.matmul(PS[:, sb * 2 + s_, 0:nq], lhsT=qk[:, kc_, ks], rhs=qzu[:, s_, 0:nq], start=True, stop=True),
                       [d_qz[C.zq], d_qk[kc_]], [], [d_ps[sb * 2 + s_]])
                    for (mkt, mj), mk in U["masks"].items():
                        if mkt != kt:
                            continue
                        PE(lambda h, s_=s_, mk=mk, mj=mj: h.matmul(PS[:, sb * 2 + s_, mj * 128:(mj + 1) * 128], lhsT=identb, rhs=mk, start=False, stop=True,
                                                                  skip_group_check=True),
                           [d_const], [], [d_ps[sb * 2 + s_]])
                A(lambda h: h.activation(out=Pb[pbi][:, :, 0:nq], in_=PS[:, sb * 2:sb * 2 + 2, 0:nq], func=AF.Exp, scale=0.125),
                  [], [d_P[pbi]], [d_ps[sb * 2], d_ps[sb * 2 + 1]])
                return pbi

            def emit_pv(C, ki, pbi):
                U = C.U
                kt, dv, vcol, valid, nj = U["ktiles"][ki], U["dv"], U["vcol"], U["valid"], C.nj
                for (bk, off, a) in C.accs:
                    s_ = a // nj
                    j = a % nj
                    if valid is not None and j not in valid[kt]:
                        continue
                    st_flag = bk not in C.started
                    C.started.add(bk)
                    PE(lambda h, bk=bk, off=off, s_=s_, j=j, st_flag=st_flag: h.matmul(
                        PS[:, 4 + bk, off:off + dv + 1], lhsT=Pb[pbi][:, s_, j * 128:(j + 1) * 128], rhs=vt[:, kt, vcol:vcol + dv + 1],
                        start=st_flag, stop=False, skip_group_check=True),
                       [d_P[pbi], d_v[kt]], [], [d_ps[4 + bk]])

            def emit_copyout(C):
                u = C.u
                asb = accs_sb[u]
                for bk in range(C.nbanks):
                    ncols = min(C.perbank, len(C.accs) - bk * C.perbank) * C.stride
                    V(lambda h, bk=bk, ncols=ncols: h.tensor_copy(out=asb[:, bk, 0:ncols], in_=PS[:, 4 + bk, 0:ncols]),
                      [], [d_accs[u][bk]], [d_ps[4 + bk]])

            def make_stages(C):
                U = C.U
                kind, nq, q0, dv, mix_chunk, sink_cols = U["kind"], U["nq"], U["q0"], U["dv"], U["mix_chunk"], U["sink_cols"]
                u, nj, accs = C.u, C.nj, C.accs
                asb = accs_sb[u]
                sm = esm[u]
                dsm = d_esm[u]
                eou = eo[u]
                deo = d_eo[u]
                eonu = eon[u]
                deon = d_eon[u]

                def parts(j):
                    a0 = accs[j]
                    a1 = accs[nj + j]
                    return (asb[:, a0[0], a0[1] + dv:a0[1] + dv + 1], asb[:, a1[0], a1[1] + dv:a1[1] + dv + 1],
                            asb[:, a0[0], a0[1]:a0[1] + dv], asb[:, a1[0], a1[1]:a1[1] + dv], d_accs[u][a0[0]], d_accs[u][a1[0]])

                def stage1():
                    if kind == "A":
                        for j in range(nj):
                            z0, z1, o0, o1, da0, da1 = parts(j)
                            V(lambda h, z0=z0, j=j: h.reciprocal(out=sm[:, 0, j:j + 1], in_=z0), [da0], [dsm])
                            V(lambda h, z1=z1, j=j: h.reciprocal(out=sm[:, 1, j:j + 1], in_=z1), [da1], [dsm])
                            V(lambda h, j=j: h.tensor_tensor(out=sm[:, 1, j:j + 1], in0=sm[:, 1, j:j + 1], in1=neglam, op=ALU.mult), [dsm, d_lam], [dsm])
                            V(lambda h, o1=o1, j=j: h.tensor_scalar(out=et[0], in0=o1, scalar1=sm[:, 1, j:j + 1], scalar2=None, op0=ALU.mult),
                              [da1, dsm], [d_et[0]])
                            V(lambda h, o0=o0, j=j: h.scalar_tensor_tensor(out=eou[:, j, :], in0=o0, scalar=sm[:, 0, j:j + 1], in1=et[0], op0=ALU.mult, op1=ALU.add),
                              [da0, dsm, d_et[0]], [deo])
                            V(lambda h, j=j: h.scalar_tensor_tensor(out=et[1], in0=eou[:, j, :], scalar=1.0, in1=eou[:, j, :], op0=ALU.mult, op1=ALU.mult,
                                                                    accum_out=sm[:, 2, j:j + 1]), [deo], [d_et[1], dsm])
                        V(lambda h: h.tensor_scalar(out=sm[:, 3, 0:nj], in0=sm[:, 2, 0:nj], scalar1=1.0 / 128, scalar2=RMS_EPS, op0=ALU.mult, op1=ALU.add),
                          [dsm], [dsm])
                        G(lambda h: h.tensor_tensor(out=sm[:, 3, 0:nj], in0=sm[:, 3, 0:nj], in1=mhalf[:, 0:nj], op=ALU.pow), [dsm, d_const], [dsm])
                        for j in range(nj):
                            V(lambda h, j=j: h.scalar_tensor_tensor(out=eonu[:, j, :], in0=eou[:, j, :], scalar=sm[:, 3, j:j + 1], in1=gA, op0=ALU.mult, op1=ALU.mult),
                              [deo, dsm, d_gA], [deon])
                    else:
                        if nj == 4:
                            av = asb[:, 0:2, 0:264].rearrange("p s (j c) -> p s j c", c=66)
                        else:
                            av = asb[:, 0, 0:264].rearrange("p (s j c) -> p s j c", s=2, j=2)
                        zv = av[:, :, :, 64]
                        ov = av[:, :, :, 0:64]
                        rz = sm[:, 0:2, 0:nj]
                        dacc = [d_accs[u][0], d_accs[u][1]] if nj == 4 else [d_accs[u][0]]
                        if sink_cols is not None:
                            V(lambda h: h.tensor_tensor(out=rz, in0=zv, in1=esink[:, sink_cols[0]:sink_cols[0] + 2, None].to_broadcast([128, 2, nj]), op=ALU.add),
                              dacc + [d_sink], [dsm])
                            V(lambda h: h.reciprocal(out=rz, in_=rz), [dsm], [dsm])
                        else:
                            V(lambda h: h.reciprocal(out=rz, in_=zv), dacc, [dsm])
                        V(lambda h: h.tensor_tensor(out=eonu[:, 0:nj, :].rearrange("p j (s c) -> p j s c", s=2),
                                                    in0=ov.rearrange("p s j c -> p j s c"),
                                                    in1=rz.rearrange("p s j -> p j s")[:, :, :, None].to_broadcast([128, nj, 2, 64]), op=ALU.mult),
                          dacc + [dsm], [deon])

                def stage2():
                    tpv = PS[:, 7, :].bitcast(BF16)[:, 0:nj * 128]
                    for j in range(nj):
                        PE(lambda h, j=j: h.transpose(out=tpv[:, j * 128:(j + 1) * 128], in_=eonu[:, j, :], identity=identb), [deon, d_const], [], [d_ps[7]])
                    V(lambda h: h.tensor_copy(out=mixT[:, mix_chunk, q0:q0 + nq], in_=tpv), [], [d_mix], [d_ps[7]])

                return stage1, stage2

            allk = list(range(NT))
            ctxk = [16, 17]
            for hd in range(4):
                streams = [(hd, 8 + hd, 0), (hd, 8 + hd, 64)]
                for qb in range(4):
                    attn_unit("A", streams, 512, qb * 512, allk, hd * 130, 128, {}, hd)
                if not last:
                    attn_unit("A", streams, 256, SEQ, ctxk, hd * 130, 128, {}, hd)
            for j in range(2):
                streams = [(4, 12, 64 * j), (5, 12, 64 * j)]
                for qb in range(4):
                    attn_unit("B", streams, 512, qb * 512, allk, 520 + j * 66, 64, {}, 4 + j)
                if not last:
                    attn_unit("B", streams, 256, SEQ, ctxk, 520 + j * 66, 64, {}, 4 + j)
            for j in range(2):
                streams = [(6, 13, 64 * j), (7, 13, 64 * j)]
                sc_ = (2 * j, 2 * j + 1)
                for qb in range(4):
                    kts = list(ctxk)
                    valid = {16: [0, 1, 2, 3], 17: [0, 1, 2, 3]}
                    masks = {}
                    for t in range(4 * qb - 1, 4 * qb + 5):
                        if t < 0 or t >= NTL:
                            continue
                        kts.append(t)
                        valid[t] = [jj for jj in range(4) if abs(t - (4 * qb + jj)) <= 1]
                        for jj in valid[t]:
                            qt = 4 * qb + jj
                            if t == qt - 1:
                                masks[(t, jj)] = mprevn
                            elif t == qt + 1:
                                masks[(t, jj)] = mnextn
                    attn_unit("C", streams, 512, qb * 512, kts, 652 + j * 66, 64, masks, 6 + j, sink_cols=sc_, valid=valid)
                if not last:
                    attn_unit("C", streams, 256, SEQ, ctxk, 652 + j * 66, 64, {}, 6 + j, sink_cols=sc_)
            units[:] = [u_ for u_ in units if u_["q0"] < SEQ] + [u_ for u_ in units if u_["q0"] >= SEQ]
            its = [(ui, ki) for ui, U in enumerate(units) for ki in range(len(U["ktiles"]))]
            ctxs = {}
            pending = []

            def flush(owner_le=None, g=None):
                keep = []
                for ent in pending:
                    if (owner_le is not None and ent[2] <= owner_le) or (g is not None and ent[0] <= g):
                        ent[1]()
                    else:
                        keep.append(ent)
                pending[:] = keep

            prep_qz(units[0], 0)
            if len(units) > 1:
                prep_qz(units[1], 1)
            pvq = []

            def pop_pv(g):
                Cp, kip, pbip = pvq.pop(0)
                emit_pv(Cp, kip, pbip)
                if kip == len(Cp.U["ktiles"]) - 1:
                    flush(owner_le=Cp.ui - 3)
                    emit_copyout(Cp)
                    s1, s2 = make_stages(Cp)
                    pending.append([g + 2, s1, Cp.ui])
                    pending.append([g + 16, s2, Cp.ui])

            for g, (ui, ki) in enumerate(its):
                if ui not in ctxs:
                    ctxs[ui] = UnitCtx(units[ui], ui)
                C = ctxs[ui]
                pbi = emit_s_exp(C, ki)
                pvq.append((C, ki, pbi))
                if ki == 0 and g > 0:
                    while len(pvq) > 1:
                        pop_pv(g)
                elif len(pvq) > 2:
                    pop_pv(g)
                n = len(C.U["ktiles"])
                if ki == min(4, n - 1) and ui + 2 < len(units):
                    prep_qz(units[ui + 2], (ui + 2) % 3)
                flush(g=g)
            g = len(its)
            while pvq:
                pop_pv(g)
            flush(owner_le=len(units))
            S.barrier()
            if debug and l == 0:
                dump("mixT", mixT, [d_mix], BF16)
            if stop_after == "p3":
                return True

            RR1.reset()
            xres = RR1.alloc([128, NT, D], F32)
            GS['xres'] = xres
            RR1x = Region(SB, RR1.base + RR1.off, R1_SIZE - RR1.off)
            RL.off = 8 * D * 2
            tmpo = [RL.alloc([128, D], F32) for _ in range(2)]
            d_tmpo = [Dep("tmpo0"), Dep("tmpo1")]
            for t in tok_tiles:
                if l == 0:
                    srcd = x_d[t * 128:(t + 1) * 128, :] if t < NTL else ctx_d[(t - NTL) * 128:(t - NTL + 1) * 128, :]
                    S.dma("sp", lambda h, t=t, srcd=srcd: h.dma_start(out=xres[:, t, :], in_=srcd), writes=[d_xres[t]])
                else:
                    S.dma("sp", lambda h, t=t: h.dma_start(out=xres[:, t, :], in_=xsp_d[t * 128:(t + 1) * 128, :]), reads=[d_xsp], writes=[d_xres[t]])
            for ti, t in enumerate(tok_tiles):
                pb0 = (ti % 2) * 2
                gt = gx1 if t < NTL else gc1
                gd = d_gates[0] if t < NTL else d_gates[1]
                ts_ = slice(t * 128, (t + 1) * 128)
                for half in range(2):
                    for kc in range(8):
                        PE(lambda h, pb0=pb0, half=half, kc=kc, ts_=ts_: h.matmul(PS[:, pb0 + half, :], lhsT=mixT[:, kc, ts_], rhs=wo[:, kc, half * 512:(half + 1) * 512],
                                                                                  start=(kc == 0), stop=(kc == 7)),
                           [d_mix, d_wo], [], [d_ps[pb0 + half]])
                tb = ti % 2
                V(lambda h, pb0=pb0, tb=tb, gt=gt: h.tensor_tensor(out=tmpo[tb], in0=PS[:, pb0:pb0 + 2, :].rearrange("p a b -> p (a b)"), in1=gt, op=ALU.mult),
                  [gd], [d_tmpo[tb]], [d_ps[pb0], d_ps[pb0 + 1]])
                G(lambda h, tb=tb, t=t: h.tensor_tensor(out=xres[:, t, :], in0=xres[:, t, :], in1=tmpo[tb], op=ALU.add),
                  [d_tmpo[tb], d_xres[t]], [d_xres[t]])
            S.barrier()
            if debug and l == 0:
                dump("xres_attn", xres, d_xres)
            if stop_after == "p4":
                return True

            h2T = hT
            d_h2 = Dep("h2T")
            groups2 = [[0, 1, 2, 3], [4, 5, 6, 7], [8, 9, 10, 11], [12, 13, 14, 15]] + ([] if last else [[16, 17]])
            rms_to_featmajor(lambda t: xres[:, t, :], groups2, scale2, 24, h2T, d_h2, lambda t: d_xres[t])
            S.barrier()
            if debug and l == 0:
                dump("h2T", h2T, [d_h2], BF16)

            RL.reset()
            ntt = len(tok_tiles)
            scr = RL.alloc([128, NT, NE], F32)
            sel = RL.alloc([128, NT, NE], F32)
            ta = RL.alloc([128, NT * 4, 3], F32)
            tb_ = RL.alloc([128, NT * 4, 2], F32)
            tc = RL.alloc([128, NT * 4], F32)
            gs = RL.alloc([128, NT * 4], F32)
            gs2 = RL.alloc([128, NT * 4], F32)
            gmax = RL.alloc([128, NT], F32)
            gsel = RL.alloc([128, NT, 4], F32)
            m1 = RL.alloc([128, NT * 4], F32)
            is1 = RL.alloc([128, NT * 4, 4], F32)
            selm = RL.alloc([128, NT * 4, 4], F32)
            m2 = RL.alloc([128, NT * 4], F32)
            top2 = RL.alloc([128, NT * 4, 4], F32)
            msk = RL.alloc([128, NT, 4, 4], F32)
            wgt = RL.alloc([128, NT, NE], F32)
            wsum = RL.alloc([128, NT], F32)
            gates = RL.alloc([128, NT, NE], F32)
            d_r = Dep("router")
            d_gt = Dep("gates")
            rps = PS[:, 0, 0:NT * NE].rearrange("p (t e) -> p t e", e=NE)
            for t in tok_tiles:
                for kc in range(8):
                    PE(lambda h, t=t, kc=kc: h.matmul(rps[:, t, :], lhsT=h2T[:, kc, t * 128:(t + 1) * 128], rhs=wrt[:, kc, :], start=(kc == 0), stop=(kc == 7)),
                       [d_h2, d_wrt], [], [d_ps[0]])
            A(lambda h: h.activation(out=scr[:, 0:ntt, :], in_=rps[:, 0:ntt, :], func=AF.Sigmoid), [], [d_r], [d_ps[0]])
            n_ = ntt
            sel4 = sel[:, 0:n_, :].rearrange("p t (g e) -> p (t g) e", g=4)
            R_ = lambda fn: V(fn, [d_r, d_rb], [d_r])
            R_(lambda h: h.tensor_tensor(out=sel[:, 0:n_, :], in0=scr[:, 0:n_, :], in1=rbias[:, None, :].to_broadcast([128, n_, NE]), op=ALU.add))
            R_(lambda h: h.tensor_tensor(out=ta[:, 0:n_ * 4, :], in0=sel4[:, :, 0:3], in1=sel4[:, :, 1:4], op=ALU.add))
            R_(lambda h: h.tensor_tensor(out=tb_[:, 0:n_ * 4, :], in0=sel4[:, :, 0:2], in1=sel4[:, :, 2:4], op=ALU.add))
            R_(lambda h: h.tensor_tensor(out=tc[:, 0:n_ * 4], in0=sel4[:, :, 0], in1=sel4[:, :, 3], op=ALU.add))
            R_(lambda h: h.tensor_reduce(out=gs[:, 0:n_ * 4], in_=ta[:, 0:n_ * 4, :], axis=AX.X, op=ALU.max))
            R_(lambda h: h.tensor_reduce(out=gs2[:, 0:n_ * 4], in_=tb_[:, 0:n_ * 4, :], axis=AX.X, op=ALU.max))
            R_(lambda h: h.tensor_tensor(out=gs[:, 0:n_ * 4], in0=gs[:, 0:n_ * 4], in1=gs2[:, 0:n_ * 4], op=ALU.max))
            R_(lambda h: h.tensor_tensor(out=gs[:, 0:n_ * 4], in0=gs[:, 0:n_ * 4], in1=tc[:, 0:n_ * 4], op=ALU.max))
            gs3 = gs[:, 0:n_ * 4].rearrange("p (t g) -> p t g", g=4)
            R_(lambda h: h.tensor_reduce(out=gmax[:, 0:n_], in_=gs3, axis=AX.X, op=ALU.max))
            R_(lambda h: h.tensor_tensor(out=gsel[:, 0:n_, :], in0=gs3, in1=gmax[:, 0:n_, None].to_broadcast([128, n_, 4]), op=ALU.is_ge))
            R_(lambda h: h.tensor_reduce(out=m1[:, 0:n_ * 4], in_=sel4, axis=AX.X, op=ALU.max))
            R_(lambda h: h.tensor_tensor(out=is1[:, 0:n_ * 4, :], in0=sel4, in1=m1[:, 0:n_ * 4, None].to_broadcast([128, n_ * 4, 4]), op=ALU.is_ge))
            R_(lambda h: h.scalar_tensor_tensor(out=selm[:, 0:n_ * 4, :], in0=is1[:, 0:n_ * 4, :], scalar=-1e9, in1=sel4, op0=ALU.mult, op1=ALU.add))
            R_(lambda h: h.tensor_reduce(out=m2[:, 0:n_ * 4], in_=selm[:, 0:n_ * 4, :], axis=AX.X, op=ALU.max))
            R_(lambda h: h.tensor_tensor(out=top2[:, 0:n_ * 4, :], in0=sel4, in1=m2[:, 0:n_ * 4, None].to_broadcast([128, n_ * 4, 4]), op=ALU.is_ge))
            top2v = top2[:, 0:n_ * 4, :].rearrange("p (t g) e -> p t g e", g=4)
            R_(lambda h: h.tensor_tensor(out=msk[:, 0:n_], in0=top2v, in1=gsel[:, 0:n_, :, None].to_broadcast([128, n_, 4, 4]), op=ALU.mult))
            R_(lambda h: h.tensor_tensor(out=wgt[:, 0:n_, :], in0=scr[:, 0:n_, :], in1=msk[:, 0:n_].rearrange("p t g e -> p t (g e)"), op=ALU.mult))
            R_(lambda h: h.tensor_reduce(out=wsum[:, 0:n_], in_=wgt[:, 0:n_, :], axis=AX.X, op=ALU.add))
            R_(lambda h: h.reciprocal(out=wsum[:, 0:n_], in_=wsum[:, 0:n_]))
            V(lambda h: h.tensor_tensor(out=gates[:, 0:n_, :], in0=wgt[:, 0:n_, :], in1=wsum[:, 0:n_, None].to_broadcast([128, n_, NE]), op=ALU.mult),
              [d_r], [d_gt])
            if debug and l == 0:
                dump("gates", gates, [d_gt])

            wg = [RL.alloc([128, 8, DEXP], BF16) for _ in range(2)]
            wu = [RL.alloc([128, 8, DEXP], BF16) for _ in range(2)]
            wds = [RL.alloc([128, 2, D], F32) for _ in range(2)]
            wdx = [RL.alloc([128, 2, D], BF16) for _ in range(2)]
            RR1x.reset()
            wdc = [RR1x.alloc([128, 2, D], BF16) for _ in range(2)]
            hid = [RR1x.alloc([128, 2, 512], BF16) for _ in range(2)]
            sg = [RR1x.alloc([128, 512], F32) for _ in range(2)]
            d_wg = [Dep("wg0"), Dep("wg1")]
            d_wu = [Dep("wu0"), Dep("wu1")]
            d_wds = [Dep("wds0"), Dep("wds1")]
            d_wdx = [Dep("wdx0"), Dep("wdx1")]
            d_wdc = [Dep("wdc0"), Dep("wdc1")]
            d_hid = [Dep("hid0"), Dep("hid1")]
            d_sg = [Dep("sg0"), Dep("sg1")]
            chunks = [(n * 512, 512) for n in range(4)] + ([] if last else [(SEQ, 256)])

            def load_expert(e):
                sl = e % 2
                S.dma("pool", lambda h: h.dma_start(out=wg[sl], in_=w_gate_d[l, e].rearrange("(kc p) n -> p kc n", p=128)), writes=[d_wg[sl]])
                S.dma("pool", lambda h: h.dma_start(out=wu[sl], in_=w_up_d[l, e].rearrange("(kc p) n -> p kc n", p=128)), writes=[d_wu[sl]])
                S.dma("sp", lambda h: h.dma_start(out=wds[sl], in_=w_down_d[l, e].rearrange("(kc p) n -> p kc n", p=128)), writes=[d_wds[sl]])
                G(lambda h: h.tensor_tensor(out=wdx[sl], in0=wds[sl], in1=gx2[:, None, :].to_broadcast([128, 2, D]), op=ALU.mult),
                  [d_wds[sl], d_gates[2]], [d_wdx[sl]])
                if not last:
                    G(lambda h: h.tensor_tensor(out=wdc[sl], in0=wds[sl], in1=gc2[:, None, :].to_broadcast([128, 2, D]), op=ALU.mult),
                      [d_wds[sl], d_gates[3]], [d_wdc[sl]])

            load_expert(0)

            def emit_gu(e, c0, cn, hb, hc):
                sl = e % 2
                bg = hc * 2
                bu = hc * 2 + 1
                for kc in range(8):
                    PE(lambda h, kc=kc: h.matmul(PS[:, bg, 0:cn], lhsT=wg[sl][:, kc, hc * 128:(hc + 1) * 128],
                                                 rhs=h2T[:, kc, c0:c0 + cn], start=(kc == 0), stop=(kc == 7)),
                       [d_wg[sl], d_h2], [], [d_ps[bg]])
                for kc in range(8):
                    PE(lambda h, kc=kc: h.matmul(PS[:, bu, 0:cn], lhsT=wu[sl][:, kc, hc * 128:(hc + 1) * 128],
                                                 rhs=h2T[:, kc, c0:c0 + cn], start=(kc == 0), stop=(kc == 7)),
                       [d_wu[sl], d_h2], [], [d_ps[bu]])
                A(lambda h: h.activation(out=sg[hc][:, 0:cn], in_=PS[:, bg, 0:cn], func=AF.Silu), [], [d_sg[hc]], [d_ps[bg]])
                V(lambda h: h.tensor_tensor(out=hid[hb][:, hc, 0:cn], in0=PS[:, bu, 0:cn], in1=sg[hc][:, 0:cn], op=ALU.mult),
                  [d_sg[hc]], [d_hid[hb]], [d_ps[bu]])

            def emit_down(e, c0, cn, hb, jts):
                sl = e % 2
                for jt in jts:
                    t = c0 // 128 + jt
                    isx = t < NTL
                    wdd = wdx[sl] if isx else wdc[sl]
                    dwd = d_wdx[sl] if isx else d_wdc[sl]
                    ob = 4 + (jt % 2) * 2
                    for half in range(2):
                        for hc in range(2):
                            PE(lambda h, half=half, hc=hc, jt=jt, wdd=wdd, ob=ob: h.matmul(
                                PS[:, ob + half, :], lhsT=hid[hb][:, hc, jt * 128:(jt + 1) * 128], rhs=wdd[:, hc, half * 512:(half + 1) * 512],
                                start=(hc == 0), stop=(hc == 1)),
                               [d_hid[hb], dwd], [], [d_ps[ob + half]])
                    V(lambda h, ob=ob, t=t: h.scalar_tensor_tensor(out=xres[:, t, :], in0=PS[:, ob:ob + 2, :].rearrange("p a b -> p (a b)"),
                                                                   scalar=gates[:, t, e:e + 1], in1=xres[:, t, :], op0=ALU.mult, op1=ALU.add),
                      [d_gt, d_xres[t]], [d_xres[t]], [d_ps[ob], d_ps[ob + 1]])

            cnt = 0
            prev = None
            for e in range(NE):
                for ci_, (c0, cn) in enumerate(chunks):
                    if ci_ == 1 and e + 1 < NE:
                        load_expert(e + 1)
                    hb = cnt % 2
                    cnt += 1
                    emit_gu(e, c0, cn, hb, 0)
                    if prev is not None:
                        pn = prev[2] // 128
                        emit_down(prev[0], prev[1], prev[2], prev[3], list(range(0, (pn + 1) // 2)))
                    emit_gu(e, c0, cn, hb, 1)
                    if prev is not None:
                        pn = prev[2] // 128
                        emit_down(prev[0], prev[1], prev[2], prev[3], list(range((pn + 1) // 2, pn)))
                    prev = (e, c0, cn, hb)
            emit_down(prev[0], prev[1], prev[2], prev[3], list(range(prev[2] // 128)))
            S.barrier()
            if debug and l == 0:
                dump("xres_moe", xres, d_xres)
            return False

        for l in range(nlayers):
            if layer(l):
                break
        xres = GS.get('xres')

        if stop_after is None:
            RL.reset()
            fing = RL.alloc([128, D], F32)
            S.dma("sp", lambda h: h.dma_start(out=fing, in_=final_g_d.partition_broadcast(128)), writes=[d_fing])
            fj = RL.alloc([128, D], BF16)
            fo = [RL.alloc([128, D], F32) for _ in range(2)]
            fs = RL.alloc([128, NTL, 2], F32)
            d_fj = Dep("fj")
            d_fo = [Dep("fo0"), Dep("fo1")]
            d_fs = [Dep("fs%d" % t) for t in range(NTL)]
            for t in range(NTL):
                A(lambda h, t=t: h.activation(out=fj, in_=xres[:, t, :], func=AF.Square, accum_out=fs[:, t, 0:1]), [d_xres[t]], [d_fj, d_fs[t]])
            A(lambda h: h.activation(out=fs[:, :, 1], in_=fs[:, :, 0], func=AF.Sqrt, scale=1.0 / D, bias=epsD[:, 0:1]), d_fs + [d_const], d_fs)
            V(lambda h: h.reciprocal(out=fs[:, :, 1], in_=fs[:, :, 1]), d_fs, d_fs)
            for t in range(NTL):
                k = t % 2
                V(lambda h, t=t, k=k: h.scalar_tensor_tensor(out=fo[k], in0=xres[:, t, :], scalar=fs[:, t, 1:2], in1=fing, op0=ALU.mult, op1=ALU.mult),
                  [d_xres[t], d_fs[t], d_fing], [d_fo[k]])
                S.dma("sp", lambda h, t=t, k=k: h.dma_start(out=out_d[t * 128:(t + 1) * 128, :], in_=fo[k]), reads=[d_fo[k]], writes=[d_out], own=d_fo[k])
        S.barrier()
        S.run()
    return nc, dbg_outs


def _consts():
    ident = np.eye(128, dtype=np.float32)
    p = np.arange(128)
    d = p % 64
    axis = d // 32
    jf = (d % 32) % 16
    inv_freq = (10000.0 ** (-(np.arange(0, 32, 2, dtype=np.float32)) / 32.0)).astype(np.float32)
    tpos = np.arange(SEQ)
    row = (tpos // 64).astype(np.float32)
    colp = (tpos % 64).astype(np.float32)
    pos = np.where(axis[:, None] == 0, row[None, :], colp[None, :]).astype(np.float32)
    ang = (pos * inv_freq[jf][:, None]).astype(np.float32)
    ropeC = np.cos(ang).astype(np.float32)
    ropeS = np.sin(ang).astype(np.float32)
    kp = np.arange(128)[:, None]
    qp = np.arange(128)[None, :]
    mprev = (qp <= kp).astype(np.float32)
    mnext = (kp <= qp).astype(np.float32)
    blk64 = ((p[:, None] // 64) == (p[None, :] // 64)).astype(np.float32) / 64.0
    return dict(ident=ident, ropeC=ropeC, ropeS=ropeS, mprev=mprev, mnext=mnext, blk64=blk64)


def _col_perm():
    q = list(range(0, 512))
    for base in (512, 768):
        for g in range(2):
            for j in range(2):
                s = base + (j * 2 + g) * 64
                q += list(range(s, s + 64))
    kA = list(range(1024, 1536))
    vA = list(range(1536, 2048))
    kB = list(range(2048, 2176))
    vB = list(range(2176, 2304))
    kC = list(range(2304, 2432))
    vC = list(range(2432, 2560))
    return np.array(q + kA + kB + kC + vA + vB + vC)


def prepare_in_maps(inputs):
    f = lambda a: np.ascontiguousarray(np.asarray(a, dtype=np.float32))
    x = f(inputs["x"]); c = f(inputs["c"]); ctx = f(inputs["ctx"]); c_ctx = f(inputs["c_ctx"])
    consts = _consts()
    perm = _col_perm()
    w_in_p = np.ascontiguousarray(f(inputs["w_in"])[:, :, perm])
    ng = np.stack([f(inputs["norm1_g"]), f(inputs["norm2_g"])], axis=1)
    ng = np.ascontiguousarray(ng.reshape(DEPTH, 2, 8, 128).transpose(3, 0, 1, 2))
    lamv = np.ascontiguousarray(np.stack([f(inputs["lam_q1"]), f(inputs["lam_k1"]), f(inputs["lam_q2"]), f(inputs["lam_k2"])], axis=1)).reshape(DEPTH, 256)
    dd = np.arange(128) % 64
    i32 = dd % 32
    permd = np.where(i32 < 16, dd + 16, dd - 16)
    qn = f(inputs["q_norm_g"]); kn = f(inputs["k_norm_g"])
    qkn = np.stack([qn[:, dd], qn[:, permd], kn[:, dd], kn[:, permd]], axis=-1)
    qkn = np.ascontiguousarray(qkn.transpose(1, 0, 2))
    shared = dict(
        w_ada=f(inputs["w_ada"]), b_ada=f(inputs["b_ada"]), ng=ng, final_g=f(inputs["final_g"]), w_in_p=w_in_p,
        w_out=f(inputs["w_out"]), lamv=lamv, subln_g=f(inputs["subln_g"]), qkn=qkn, sink=f(inputs["sink"]),
        w_router=f(inputs["w_router"]), router_bias=f(inputs["router_bias"]), w_gate=f(inputs["w_gate"]), w_up=f(inputs["w_up"]),
        w_down=f(inputs["w_down"]), **consts)
    in_maps = []
    for b in range(8):
        cc = np.stack([c[b].reshape(8, 128).T, c_ctx.reshape(8, 128).T], axis=-1)
        m = dict(shared)
        m.update(x=np.ascontiguousarray(x[b]), ctx=np.ascontiguousarray(ctx[b]), cc=np.ascontiguousarray(cc))
        in_maps.append(m)
    return in_maps


_CACHE = {}


def kernel(**inputs):
    in_maps = prepare_in_maps(inputs)
    if "nc" not in _CACHE:
        _CACHE["nc"] = build_program()[0]
    nc = _CACHE["nc"]
    res = run_bass_kernel_spmd(nc, in_maps, core_ids=list(range(8)))
    out = np.stack([np.asarray(r["out"], dtype=np.float32) for r in res.results], axis=0)
    return out
```
